# Optimizing a Trainium2 kernel written in Bass

```python
import math
import jax, jax.numpy as jnp
from jax import lax
import numpy as np

D_MODEL = 1024
BATCH = 8
SEQ = 2048
DEPTH = 1

POOL_WINDOWS = (2, 4, 8, 16)
POOL_GROUPS = len(POOL_WINDOWS)
POOL_WIDTH = D_MODEL // 2
POOL_GROUP_DIM = POOL_WIDTH // POOL_GROUPS
HEAD_DIM = 64
N_Q_HEADS = D_MODEL // 128
N_KV_HEADS = 2
Q_PER_KV = N_Q_HEADS // N_KV_HEADS
ATTN_WIDTH = N_Q_HEADS * HEAD_DIM
KV_WIDTH = N_KV_HEADS * HEAD_DIM
WINDOW = 128
BLOCK = WINDOW
ROPE_THETA = 10000.0
N_BRANCHES = 2
IN_COLS = POOL_WIDTH + ATTN_WIDTH + 2 * KV_WIDTH + N_BRANCHES * D_MODEL
SPLITS = (POOL_WIDTH, POOL_WIDTH + ATTN_WIDTH, POOL_WIDTH + ATTN_WIDTH + KV_WIDTH,
          POOL_WIDTH + ATTN_WIDTH + 2 * KV_WIDTH)
N_GROUPS = 4
EXPERTS_PER_GROUP = 8
N_EXPERTS = N_GROUPS * EXPERTS_PER_GROUP
TOP_K = 2
D_EXPERT = D_MODEL // 2
MOE_BLOCK = 128
ALPHA = (2 * DEPTH) ** 0.25
BETA = (8 * DEPTH) ** -0.25
LN_EPS = 1e-5

kernel_name = "hybrid_pool_bandattn_hiermoe_encoder"


def layer_norm(x, g, b):
    xf = x.astype(jnp.float32)
    mu = jnp.mean(xf, axis=-1, keepdims=True)
    var = jnp.mean(jnp.square(xf - mu), axis=-1, keepdims=True)
    return ((xf - mu) * lax.rsqrt(var + LN_EPS) * g.astype(jnp.float32)
            + b.astype(jnp.float32)).astype(x.dtype)


def pool_mixer(u, w_mix, b_mix, scale):
    B, S, _ = u.shape
    ug = u.reshape(B, S, POOL_GROUPS, POOL_GROUP_DIM).astype(jnp.float32)
    cs = jnp.concatenate([jnp.zeros((B, 1, POOL_GROUPS, POOL_GROUP_DIM), jnp.float32),
                          jnp.cumsum(ug, axis=1)], axis=1)
    t = jnp.arange(S)
    outs = []
    for g, w in enumerate(POOL_WINDOWS):
        lo = jnp.clip(t - w // 2, 0, S)
        hi = jnp.clip(t - w // 2 + w, 0, S)
        csg = cs[:, :, g]
        mean = (csg[:, hi] - csg[:, lo]) / (hi - lo).astype(jnp.float32)[None, :, None]
        outs.append(mean - ug[:, :, g])
    pooled = jnp.stack(outs, axis=2).astype(u.dtype)
    mixed = jnp.einsum('bsgc,gcd->bsgd', pooled, w_mix) + b_mix
    return mixed.reshape(B, S, POOL_WIDTH) * scale


def rope(t, cos, sin):
    t1, t2 = jnp.split(t, 2, axis=-1)
    return jnp.concatenate([t1 * cos - t2 * sin, t2 * cos + t1 * sin], axis=-1)


def banded_gqa(q, k, v, sink):
    B, S, _ = q.shape
    nb = S // BLOCK
    q = q.reshape(B, S, N_Q_HEADS, HEAD_DIM)
    k = k.reshape(B, S, N_KV_HEADS, HEAD_DIM)
    v = v.reshape(B, S, N_KV_HEADS, HEAD_DIM)
    pos = jnp.arange(S, dtype=jnp.float32)
    inv = ROPE_THETA ** (-jnp.arange(0, HEAD_DIM, 2, dtype=jnp.float32) / HEAD_DIM)
    ang = pos[:, None] * inv[None, :]
    cos = jnp.cos(ang)[None, :, None, :].astype(q.dtype)
    sin = jnp.sin(ang)[None, :, None, :].astype(q.dtype)
    q = rope(q, cos, sin)
    k = rope(k, cos, sin)
    qb = q.reshape(B, nb, BLOCK, N_KV_HEADS, Q_PER_KV, HEAD_DIM)

    def band(t):
        tp = jnp.pad(t, ((0, 0), (WINDOW, WINDOW), (0, 0), (0, 0)))
        tp = tp.reshape(B, nb + 2, BLOCK, N_KV_HEADS, HEAD_DIM)
        return jnp.concatenate([tp[:, :-2], tp[:, 1:-1], tp[:, 2:]], axis=2)

    kb, vb = band(k), band(v)
    s = jnp.einsum('bnqhgd,bnkhd->bnhgqk', qb, kb).astype(jnp.float32) * (HEAD_DIM ** -0.5)
    qpos = jnp.arange(nb)[:, None] * BLOCK + jnp.arange(BLOCK)[None, :]
    kpos = jnp.arange(nb)[:, None] * BLOCK - WINDOW + jnp.arange(3 * BLOCK)[None, :]
    rel = kpos[:, None, :] - qpos[:, :, None]
    valid = (jnp.abs(rel) <= WINDOW) & (kpos[:, None, :] >= 0) & (kpos[:, None, :] < S)
    s = jnp.where(valid[None, :, None, None], s, -1e30)
    sink_l = jnp.broadcast_to(sink.astype(jnp.float32).reshape(1, 1, N_KV_HEADS, Q_PER_KV, 1, 1),
                              s.shape[:-1] + (1,))
    p = jax.nn.softmax(jnp.concatenate([s, sink_l], axis=-1), axis=-1)[..., :-1]
    o = jnp.einsum('bnhgqk,bnkhd->bnqhgd', p.astype(vb.dtype), vb)
    return o.reshape(B, S, ATTN_WIDTH)


def token_mixer(x, w_in, b_in, w_pool_mix, b_pool_mix, pool_scale, attn_sink,
                w_pool_br, w_attn_br, w_o):
    B, S, D = x.shape
    h = x @ w_in + b_in
    u_pool, q, k, v, g_logits = jnp.split(h, SPLITS, axis=-1)
    y_pool = pool_mixer(u_pool, w_pool_mix, b_pool_mix, pool_scale) @ w_pool_br
    y_attn = banded_gqa(q, k, v, attn_sink) @ w_attn_br
    g = jax.nn.sigmoid(g_logits.astype(jnp.float32)).astype(x.dtype).reshape(B, S, N_BRANCHES, D)
    merged = g[:, :, 0] * y_pool + g[:, :, 1] * y_attn
    return merged @ w_o


def hier_moe(x, w_rg, b_rg, w_re, b_re, w_gate, w_up, w_down):
    B, S, D = x.shape
    T = B * S
    xt = x.reshape(T, D)
    xf = xt.astype(jnp.float32)
    gl = xf @ w_rg.astype(jnp.float32) + b_rg.astype(jnp.float32)
    gp = jax.nn.softmax(gl, axis=-1)
    g_idx = jnp.argmax(gl, axis=-1)
    g_w = jnp.take_along_axis(gp, g_idx[:, None], axis=1)[:, 0]
    el = (xf @ w_re.astype(jnp.float32) + b_re.astype(jnp.float32)).reshape(T, N_GROUPS, EXPERTS_PER_GROUP)
    sel = jnp.take_along_axis(el, g_idx[:, None, None], axis=1)[:, 0]
    ep = jax.nn.softmax(sel, axis=-1)
    top_v, top_i = lax.top_k(ep, TOP_K)
    top_v = top_v / jnp.sum(top_v, axis=-1, keepdims=True)
    weights = (g_w[:, None] * top_v).reshape(-1)
    eid = (g_idx[:, None] * EXPERTS_PER_GROUP + top_i).reshape(-1).astype(jnp.int32)
    tok = jnp.repeat(jnp.arange(T, dtype=jnp.int32), TOP_K)
    A = T * TOP_K
    order = jnp.argsort(eid, stable=True)
    se = eid[order]
    counts = jnp.bincount(eid, length=N_EXPERTS)
    starts = jnp.cumsum(counts) - counts
    pcounts = (counts + MOE_BLOCK - 1) // MOE_BLOCK * MOE_BLOCK
    pends = jnp.cumsum(pcounts)
    pstarts = pends - pcounts
    dest = pstarts[se] + (jnp.arange(A) - starts[se])
    n_blocks = -(-A // MOE_BLOCK) + N_EXPERTS
    P = n_blocks * MOE_BLOCK
    row_tok = jnp.full((P,), T, jnp.int32).at[dest].set(tok[order])
    row_w = jnp.zeros((P,), jnp.float32).at[dest].set(weights[order])
    blk_e = jnp.minimum(jnp.searchsorted(pends, jnp.arange(n_blocks) * MOE_BLOCK, side='right'),
                        N_EXPERTS - 1)
    xpad = jnp.concatenate([xt, jnp.zeros((1, D), xt.dtype)], axis=0)
    xin = xpad[row_tok].reshape(n_blocks, MOE_BLOCK, D)

    def expert_block(args):
        xb, e = args
        hb = jax.nn.silu(xb @ w_gate[e]) * (xb @ w_up[e])
        return hb @ w_down[e]

    yb = lax.map(expert_block, (xin, blk_e)).reshape(P, D)
    y = jax.ops.segment_sum(yb.astype(jnp.float32) * row_w[:, None], row_tok, num_segments=T + 1)[:T]
    return y.astype(x.dtype).reshape(B, S, D)


def setup_inputs(seed: int = 0) -> dict:
    key = jax.random.key(seed)
    ks = jax.random.split(key, 24)
    n = jax.random.normal
    f32 = jnp.float32
    C = POOL_GROUP_DIM
    return {
        "x": n(ks[0], (BATCH, SEQ, D_MODEL), f32),
        "w_in": n(ks[1], (DEPTH, D_MODEL, IN_COLS), f32) * D_MODEL ** -0.5,
        "b_in": 0.02 * n(ks[2], (DEPTH, IN_COLS), f32),
        "w_pool_mix": n(ks[3], (DEPTH, POOL_GROUPS, C, C), f32) * C ** -0.5,
        "b_pool_mix": 0.02 * n(ks[4], (DEPTH, POOL_GROUPS, C), f32),
        "pool_scale": 1.0 + 0.1 * n(ks[5], (DEPTH, POOL_WIDTH), f32),
        "attn_sink": 0.5 * n(ks[6], (DEPTH, N_Q_HEADS), f32),
        "w_pool_br": n(ks[7], (DEPTH, POOL_WIDTH, D_MODEL), f32) * POOL_WIDTH ** -0.5,
        "w_attn_br": n(ks[8], (DEPTH, ATTN_WIDTH, D_MODEL), f32) * ATTN_WIDTH ** -0.5,
        "w_o": n(ks[9], (DEPTH, D_MODEL, D_MODEL), f32) * D_MODEL ** -0.5 * BETA,
        "ln1_g": 1.0 + 0.02 * n(ks[10], (DEPTH, D_MODEL), f32),
        "ln1_b": 0.02 * n(ks[11], (DEPTH, D_MODEL), f32),
        "w_router_group": n(ks[12], (DEPTH, D_MODEL, N_GROUPS), f32) * D_MODEL ** -0.5,
        "b_router_group": 0.01 * n(ks[13], (DEPTH, N_GROUPS), f32),
        "w_router_expert": n(ks[14], (DEPTH, D_MODEL, N_EXPERTS), f32) * D_MODEL ** -0.5,
        "b_router_expert": 0.01 * n(ks[15], (DEPTH, N_EXPERTS), f32),
        "w_gate": n(ks[16], (DEPTH, N_EXPERTS, D_MODEL, D_EXPERT), f32) * D_MODEL ** -0.5,
        "w_up": n(ks[17], (DEPTH, N_EXPERTS, D_MODEL, D_EXPERT), f32) * D_MODEL ** -0.5,
        "w_down": n(ks[18], (DEPTH, N_EXPERTS, D_EXPERT, D_MODEL), f32) * D_EXPERT ** -0.5 * BETA,
        "ln2_g": 1.0 + 0.02 * n(ks[19], (DEPTH, D_MODEL), f32),
        "ln2_b": 0.02 * n(ks[20], (DEPTH, D_MODEL), f32),
    }


def reference(x, w_in, b_in, w_pool_mix, b_pool_mix, pool_scale, attn_sink,
              w_pool_br, w_attn_br, w_o, ln1_g, ln1_b, w_router_group, b_router_group,
              w_router_expert, b_router_expert, w_gate, w_up, w_down, ln2_g, ln2_b):
    for l in range(DEPTH):
        mix = token_mixer(x, w_in[l], b_in[l], w_pool_mix[l], b_pool_mix[l], pool_scale[l],
                          attn_sink[l], w_pool_br[l], w_attn_br[l], w_o[l])
        x = layer_norm(ALPHA * x + mix, ln1_g[l], ln1_b[l])
        ffn = hier_moe(x, w_router_group[l], b_router_group[l], w_router_expert[l],
                       b_router_expert[l], w_gate[l], w_up[l], w_down[l])
        x = layer_norm(ALPHA * x + ffn, ln2_g[l], ln2_b[l])
    return x
```

```python
import contextlib
import numpy as np
import concourse.bass as bass
import concourse.mybir as mybir
from concourse.bass_utils import run_bass_kernel_spmd

F32 = mybir.dt.float32
BF16 = mybir.dt.bfloat16
AF = mybir.ActivationFunctionType
ALU = mybir.AluOpType
AX = mybir.AxisListType

S_LEN = 2048
D = 1024
NT = 16
NS = 4
N_EXP = 32
ALPHA = 2.0 ** 0.25
LN_EPS = 1e-5
N_CORES = 8
N_INCH = 31
SLOT_B = 256
SLOT_SHIFT = 8
SUBS = SLOT_B // 128
N_OVF = (2 * S_LEN) // SLOT_B
N_SLOTS = N_EXP + N_OVF
I32 = mybir.dt.int32

COMPUTE = ("pe", "act", "dve", "pool")
ENGS = ("pe", "act", "dve", "pool", "sp")
PAGE = 2048


class View:
    __slots__ = ("ap", "arena", "ivals")

    def __init__(self, ap, arena, ivals):
        self.ap = ap
        self.arena = arena
        self.ivals = ivals


def _ivals(shape, esize, base, idx):
    nd = len(shape)
    rngs = []
    for d in range(nd):
        if d < len(idx):
            i = idx[d]
            if isinstance(i, int):
                rngs.append((i, i + 1))
            else:
                a = 0 if i.start is None else i.start
                b = shape[d] if i.stop is None else i.stop
                rngs.append((a, b))
        else:
            rngs.append((0, shape[d]))
    strides = [0] * nd
    s = esize
    for d in range(nd - 1, -1, -1):
        strides[d] = s
        s *= shape[d]
    tail = nd
    while tail > 0 and rngs[tail - 1] == (0, shape[tail - 1]):
        tail -= 1
    out = []
    if tail == 0:
        return [(base, base + s)]
    lead = rngs[: tail - 1]
    a, b = rngs[tail - 1]
    blk_lo = a * strides[tail - 1]
    blk_hi = b * strides[tail - 1]
    cnt = 1
    for (x, y) in lead:
        cnt *= (y - x)
    if cnt > 64:
        lo = base + sum(r[0] * strides[d] for d, r in enumerate(lead)) + blk_lo
        hi = base + sum((r[1] - 1) * strides[d] for d, r in enumerate(lead)) + blk_hi
        return [(lo, hi)]

    def rec(d, off):
        if d == tail - 1:
            out.append((base + off + blk_lo, base + off + blk_hi))
            return
        for i in range(lead[d][0], lead[d][1]):
            rec(d + 1, off + i * strides[d])
    rec(0, 0)
    return out


class Buf:
    def __init__(self, arena_name, ap_full, shape, esize, base_bytes):
        self.arena = arena_name
        self.ap_full = ap_full
        self.shape = tuple(shape)
        self.esize = esize
        self.base = base_bytes

    def v(self, *idx, p=None):
        full = (slice(None) if p is None else p,) + tuple(idx)
        ap = self.ap_full[full]
        if self.arena == "ps":
            return View(ap, self.arena, [(self.base, self.base + 2048)])
        return View(ap, self.arena, _ivals(self.shape, self.esize, self.base, idx))


class Sched:
    def __init__(self, nc, n_dma_sems=32):
        self.nc = nc
        self.streams = {e: [] for e in ENGS}
        self.cnt = {e: 0 for e in COMPUTE}
        self.pages = {}
        self.waited = {}
        self.n_dma_sems = n_dma_sems
        self.dma_issued = 0
        self.dma_slot_val = [0] * n_dma_sems

    def _touch(self, view, is_write, deps, own_sem=None):
        ps = view.arena == "ps"
        for (lo, hi) in view.ivals:
            for pg in range(lo // PAGE, (hi - 1) // PAGE + 1):
                d = self.pages.get((view.arena, pg))
                if not d:
                    continue
                for (rlo, rhi, sem, kind), val in d.items():
                    if rlo < hi and lo < rhi and (is_write or kind == "w" or (ps and sem != own_sem)):
                        deps.append((sem, val))

    def _record(self, view, is_write, token):
        sem, val = token
        kind = "w" if is_write else "r"
        for (lo, hi) in view.ivals:
            for pg in range(lo // PAGE, (hi - 1) // PAGE + 1):
                d = self.pages.setdefault((view.arena, pg), {})
                if is_write:
                    dead = [k for k in d if k[0] >= lo and k[1] <= hi]
                    for k in dead:
                        del d[k]
                d[(lo, hi, sem, kind)] = val

    def op(self, eng, fn, reads=(), writes=(), dma=False):
        deps = []
        own = None if dma else (eng,)
        for v in reads:
            self._touch(v, False, deps, own)
        for v in writes:
            self._touch(v, True, deps, own)
        if dma:
            slot = self.dma_issued % self.n_dma_sems
            self.dma_issued += 1
            prev = self.dma_slot_val[slot]
            if prev > 0:
                deps.append((("dma", slot), prev))
            self.dma_slot_val[slot] = prev + 16
            token = (("dma", slot), prev + 16)
            inc = 16
        else:
            self.cnt[eng] += 1
            token = ((eng,), self.cnt[eng])
            inc = 1
        need = {}
        for sem, val in deps:
            if (not dma) and eng == "pe" and sem == ("pe",):
                continue
            if need.get(sem, 0) < val:
                need[sem] = val
        waits = []
        for sem, val in need.items():
            if self.waited.get((eng, sem), 0) >= val:
                continue
            self.waited[(eng, sem)] = val
            waits.append((sem, val))
        self.streams[eng].append((waits, fn, token, inc))
        for v in reads:
            self._record(v, False, token)
        for v in writes:
            self._record(v, True, token)
        return token

    def emit(self, es, final_wait_eng="sp"):
        nc = self.nc
        sems = {}
        for e in COMPUTE:
            sems[(e,)] = es.enter_context(nc.semaphore("s_" + e))
        for i in range(self.n_dma_sems):
            sems[("dma", i)] = es.enter_context(nc.semaphore("s_dma%d" % i))
        block = es.enter_context(nc.Block())
        finals = []
        for e in COMPUTE:
            if self.cnt[e] > 0:
                finals.append(((e,), self.cnt[e]))
        for i in range(self.n_dma_sems):
            if self.dma_slot_val[i] > 0:
                finals.append((("dma", i), self.dma_slot_val[i]))

        def make(engname):
            stream = self.streams[engname]

            def body(eng):
                for waits, fn, token, inc in stream:
                    for sem, val in waits:
                        eng.wait_ge(sems[sem], val)
                    ins = fn(eng)
                    ins.then_inc(sems[token[0]], inc)
                if engname == final_wait_eng:
                    for sem, val in finals:
                        eng.wait_ge(sems[sem], val)
            return body

        block.tensor(make("pe"))
        block.scalar(make("act"))
        block.vector(make("dve"))
        block.gpsimd(make("pool"))
        block.sync(make("sp"))


ARENA_WORDS = 51200


def build_program(stop=99, dbg=False):
    nc = bass.Bass("TRN2", target_bir_lowering=False)
    dbg_list = []

    def din(name, shape, dt=F32):
        return nc.dram_tensor(name, list(shape), dt, kind="ExternalInput").ap()

    d_xT = din("xT", [D, S_LEN])
    d_x = din("x_tok", [S_LEN, D])
    d_win = din("w_in_r", [N_INCH, 128, 8, 128])
    d_bin = din("b_in_r", [128, N_INCH])
    d_bv = din("bv_bc", [128, 128])
    d_cos = din("cosT", [128, S_LEN])
    d_sin = din("sinT", [128, S_LEN])
    d_ec = din("ec", [128, 4, 16])
    d_wmix = din("wmix_r", [128, 4, 128])
    d_bmix = din("bmix_r", [128, 4])
    d_pscale = din("pscale_r", [128, 4])
    d_sink = din("sink_bc", [128, 8])
    d_wpb = din("wpb_r", [128, 4, D])
    d_wab = din("wab_r", [128, 4, D])
    d_wo = din("wo_r", [128, 8, D])
    d_ln1g = din("ln1g_bc", [128, D])
    d_ln1b = din("ln1b_bc", [128, D])
    d_ln2g = din("ln2g_bc", [128, D])
    d_ln2b = din("ln2b_bc", [128, D])
    d_wr = din("wr_r", [128, 8, 36])
    d_br = din("br_bc", [128, 36])
    if stop >= 5:
        d_wg = din("wg_r", [N_EXP, 128, 8, 512])
        d_wu = din("wu_r", [N_EXP, 128, 8, 512])
        d_wd = din("wd_r", [N_EXP, 128, 4, D])
        d_wg_rows = d_wg.rearrange("e p k m -> (e p) (k m)")
        d_wu_rows = d_wu.rearrange("e p k m -> (e p) (k m)")
        d_wd_rows = d_wd.rearrange("e p k m -> (e p) (k m)")
    d_xin = nc.dram_tensor("xin_scratch", [N_SLOTS * SLOT_B, D], BF16, kind="Internal").ap()
    d_yb = nc.dram_tensor("yb_scratch", [N_SLOTS * SLOT_B, D], F32, kind="Internal").ap()
    d_out = nc.dram_tensor("out", [S_LEN, D], F32, kind="ExternalOutput").ap()
    d_x2s = nc.dram_tensor("x2_scratch", [S_LEN, D], F32, kind=("ExternalOutput" if dbg else "Internal")).ap()

    es = contextlib.ExitStack()
    with es:
        arena = es.enter_context(nc.sbuf_tensor("arena", [128, ARENA_WORDS], F32))
        banks_t = [es.enter_context(nc.psum_tensor("ps%d" % i, [128, 512], F32)) for i in range(8)]
        S = Sched(nc)

        def buf(off, shape, dt=F32):
            n = int(np.prod(shape))
            if dt == F32:
                words = n
                ap = arena[:, off:off + words]
                esize = 4
            elif dt == I32:
                words = n
                ap = arena[:, off:off + words].bitcast(I32)
                esize = 4
            else:
                assert n % 2 == 0
                words = n // 2
                ap = arena[:, off:off + words].bitcast(BF16)
                esize = 2
            if len(shape) > 1:
                names = " ".join("d%d" % i for i in range(len(shape)))
                kw = {"d%d" % i: shape[i] for i in range(1, len(shape))}
                ap = ap.rearrange("p (%s) -> p %s" % (names, names), **kw)
            return Buf("sb", ap, shape, esize, off * 4), off + words

        def psbuf(i, shape, dt=F32):
            n_ = int(np.prod(shape))
            if dt == F32:
                ap = banks_t[i][:, 0:n_]
                esize = 4
            else:
                ap = banks_t[i][:].bitcast(BF16)[:, 0:n_]
                esize = 2
            if len(shape) > 1:
                names = " ".join("d%d" % j for j in range(len(shape)))
                kw = {"d%d" % j: shape[j] for j in range(1, len(shape))}
                ap = ap.rearrange("p (%s) -> p %s" % (names, names), **kw)
            return Buf("ps", ap, shape, esize, i * 2048)

        PSF = [psbuf(i, [512]) for i in range(8)]
        PSB = [psbuf(i, [1024], BF16) for i in range(8)]
        ps_rr = [0]

        def next_bank():
            i = ps_rr[0] % 8
            ps_rr[0] += 1
            return i

        def dview(ap, name, lo, hi):
            return View(ap, name, [(lo, hi)])

        def mm(out, lhsT, rhs, start, stop):
            S.op("pe", lambda e: e.matmul(out.ap, lhsT.ap, rhs.ap, start=start, stop=stop),
                 reads=[lhsT, rhs], writes=[out])

        def tr(out, in_, ident):
            S.op("pe", lambda e: e.transpose(out.ap, in_.ap, ident.ap), reads=[in_, ident], writes=[out])

        def act(out, in_, func, bias=0.0, scale=1.0, accum=None, eng="act"):
            rd = [in_]
            b = bias
            s = scale
            if isinstance(bias, View):
                rd.append(bias)
                b = bias.ap
            if isinstance(scale, View):
                rd.append(scale)
                s = scale.ap
            wr = [out]
            if accum is not None:
                wr.append(accum)
                S.op(eng, lambda e: e.activation(out=out.ap, in_=in_.ap, func=func, bias=b, scale=s,
                                                 accum_out=accum.ap), reads=rd, writes=wr)
            else:
                S.op(eng, lambda e: e.activation(out=out.ap, in_=in_.ap, func=func, bias=b, scale=s),
                     reads=rd, writes=wr)

        def tt(eng, out, in0, in1, op):
            S.op(eng, lambda e: e.tensor_tensor(out=out.ap, in0=in0.ap, in1=in1.ap, op=op),
                 reads=[in0, in1], writes=[out])

        def ts(eng, out, in0, s1, op0, s2=None, op1=None):
            rd = [in0]
            a1 = s1
            a2 = s2
            if isinstance(s1, View):
                rd.append(s1)
                a1 = s1.ap
            if isinstance(s2, View):
                rd.append(s2)
                a2 = s2.ap
            if op1 is None:
                S.op(eng, lambda e: e.tensor_scalar(out=out.ap, in0=in0.ap, scalar1=a1, scalar2=None, op0=op0),
                     reads=rd, writes=[out])
            else:
                S.op(eng, lambda e: e.tensor_scalar(out=out.ap, in0=in0.ap, scalar1=a1, scalar2=a2, op0=op0, op1=op1),
                     reads=rd, writes=[out])

        def stt(out, in0, scalar, in1, op0, op1):
            rd = [in0, in1]
            sc = scalar
            if isinstance(scalar, View):
                rd.append(scalar)
                sc = scalar.ap
            S.op("dve", lambda e: e.scalar_tensor_tensor(out=out.ap, in0=in0.ap, scalar=sc, in1=in1.ap, op0=op0, op1=op1),
                 reads=rd, writes=[out])

        def cp(eng, out, in_):
            if eng == "act":
                S.op(eng, lambda e: e.copy(out=out.ap, in_=in_.ap), reads=[in_], writes=[out])
            else:
                S.op(eng, lambda e: e.tensor_copy(out=out.ap, in_=in_.ap), reads=[in_], writes=[out])

        def memset(eng, out, val):
            S.op(eng, lambda e: e.memset(out.ap, val), writes=[out])

        def dma(eng, out, in_):
            S.op(eng, lambda e: e.dma_start(out=out.ap, in_=in_.ap), reads=[in_], writes=[out], dma=True)

        A0, B0, C0, D0, E0, F0 = 0, 16384, 28672, 36864, 45056, 50176
        o = F0
        ident_f, o = buf(o, [128])
        ident_b, o = buf(o, [128], BF16)
        wmix, o = buf(o, [4, 128], BF16)
        wr_f, o = buf(o, [8, 36])
        b_in, o = buf(o, [N_INCH])
        bmix, o = buf(o, [4])
        pscale, o = buf(o, [4])
        bms, o = buf(o, [4])
        esink, o = buf(o, [8])
        br_bc, o = buf(o, [36])
        ec, o = buf(o, [4, 16])
        den, o = buf(o, [4])
        rden, o = buf(o, [4])
        assert o <= ARENA_WORDS, o
        xT, _ = buf(D0, [8, S_LEN], BF16)
        mixT, _ = buf(C0, [4, S_LEN], BF16)
        oT, _ = buf(C0 + 4096, [4, S_LEN], BF16)
        hT = [buf(C0 + 1024 * i, [4, 512], BF16)[0] for i in range(8)]
        o = B0
        wch = []
        for i in range(4):
            b_, o = buf(o, [8, 128], BF16)
            wch.append(b_)
        wpb, o = buf(o, [4, D], BF16)
        wab, o = buf(o, [4, D], BF16)
        wo, o = buf(o, [8, D], BF16)
        mT, o = buf(o, [8, 512], BF16)
        assert o == C0
        wexp = []
        o = B0
        for i in range(2):
            g_, o = buf(o, [8, 512], BF16)
            u_, o = buf(o, [8, 512], BF16)
            d_, o = buf(o, [4, D], BF16)
            wexp.append((g_, u_, d_))
        o = A0
        PADW = S_LEN + 16
        uT, o = buf(o, [PADW])
        sA, o = buf(o, [PADW])
        sB, o = buf(o, [PADW])
        cosT, o = buf(o, [S_LEN])
        sinT, o = buf(o, [S_LEN])
        qT, o = buf(o, [4, S_LEN], BF16)
        kT, o = buf(o, [S_LEN], BF16)
        otok = []
        for i in range(2):
            b_, o = buf(o, [512], BF16)
            otok.append(b_)
        assert o <= B0, o
        o = A0
        sg0, o = buf(o, [512])
        sg1, o = buf(o, [512])
        t0b, o = buf(o, [512])
        t1b, o = buf(o, [512])
        xtok = []
        abuf = []
        for i in range(4):
            b_, o = buf(o, [D])
            xtok.append(b_)
        for i in range(2):
            b_, o = buf(o, [D])
            abuf.append(b_)
        x2Tf, o = buf(o, [8, 128])
        ln1g, o = buf(o, [D])
        ln1b, o = buf(o, [D])
        stats, o = buf(o, [2, 6])
        mv, o = buf(o, [2])
        rstd, o = buf(o, [1])
        lg, o = buf(o, [36])
        rt, o = buf(o, [64])
        assert o <= B0, o
        o = E0 + 1536
        OH1f, o = buf(o, [NT, 32])
        OH2f, o = buf(o, [NT, 32])
        w12, o = buf(o, [2, NT])
        assert o <= F0, o
        o = A0
        prefix, o = buf(o, [NT, 32])
        slotpos, o = buf(o, [NT, 32])
        tmp512, o = buf(o, [NT, 32])
        ind, o = buf(o, [NT, 32], BF16)
        ones_bf, o = buf(o, [128], BF16)
        stri_bf, o = buf(o, [128], BF16)
        cnt, o = buf(o, [32])
        cnt_i, o = buf(o, [32], I32)
        nb_i, o = buf(o, [32], I32)
        nb_f, o = buf(o, [32])
        pendb, o = buf(o, [32])
        pstart, o = buf(o, [32])
        zeros32, o = buf(o, [32])
        junk32, o = buf(o, [32])
        es_f, o = buf(o, [N_SLOTS])
        widx_f, o = buf(o, [N_SLOTS])
        widx_i, o = buf(o, [N_SLOTS], I32)
        d1f, o = buf(o, [NT])
        d2f, o = buf(o, [NT])
        d1i, o = buf(o, [NT], I32)
        d2i, o = buf(o, [NT], I32)
        cm_f, o = buf(o, [32])
        basep, o = buf(o, [32])
        basep_i, o = buf(o, [32], I32)
        pidx, o = buf(o, [1])
        pidx_i, o = buf(o, [1], I32)
        o += (-o) % 2
        o_rg = o
        rg1, rg2 = [], []
        for i in range(2):
            b_, o_rg = buf(o_rg, [D])
            rg1.append(b_)
            b_, o_rg = buf(o_rg, [D])
            rg2.append(b_)
        xr = []
        for i in range(2):
            b_, o = buf(o, [D])
            xr.append(b_)
        xb, xbT = [], []
        for i in range(4):
            b_, o = buf(o, [D], BF16)
            xb.append(b_)
        for i in range(4):
            b_, o = buf(o, [8, 128], BF16)
            xbT.append(b_)
        assert o <= A0 + 8704, o
        o = C0
        hTs, hTs4, ybs = [], [], []
        for i in range(4):
            h4, _ = buf(o, [4, 128], BF16)
            b_, o = buf(o, [512], BF16)
            hTs.append(b_)
            hTs4.append(h4)
        for i in range(4):
            b_, o = buf(o, [D])
            ybs.append(b_)
        assert o <= D0, o
        wexp2 = []
        o = B0
        for i in range(2):
            g_, o = buf(o, [4096], BF16)
            u_, o = buf(o, [4096], BF16)
            d_, o = buf(o, [4096], BF16)
            wexp2.append((g_, u_, d_))
        o = A0 + 8704
        g_, o = buf(o, [8, 512], BF16)
        u_, o = buf(o, [8, 512], BF16)
        d_, o = buf(o, [4, D], BF16)
        wexp.append((g_, u_, d_))
        assert o <= B0, o
        o = A0 + 8704
        g_, o = buf(o, [4096], BF16)
        u_, o = buf(o, [4096], BF16)
        d_, o = buf(o, [4096], BF16)
        wexp2.append((g_, u_, d_))
        o = D0
        g_, o = buf(o, [8, 512], BF16)
        u_, o = buf(o, [8, 512], BF16)
        d_, o = buf(o, [4, D], BF16)
        wexp.append((g_, u_, d_))
        assert o <= E0, o
        o = D0
        g_, o = buf(o, [4096], BF16)
        u_, o = buf(o, [4096], BF16)
        d_, o = buf(o, [4096], BF16)
        wexp2.append((g_, u_, d_))
        NWS = 4
        wexp = [wexp[0], wexp[1], wexp[3], wexp[2]]
        wexp2 = [wexp2[0], wexp2[1], wexp2[3], wexp2[2]]
        xr.append(buf(C0 + 1024, [D])[0])
        xr.append(buf(C0 + 2048, [D])[0])
        o = E0
        vaug, o = buf(o, [NT, 2, 65], BF16)
        pT = []
        for i in range(2):
            b_, o = buf(o, [3, 512], BF16)
            pT.append(b_)
        pooled, o = buf(o, [S_LEN], BF16)
        rtmp1, o = buf(o, [512])
        rtmp2, o = buf(o, [512])
        bv_bc, o = buf(o, [128])
        assert o <= F0, o
        o = E0
        sl = []
        for i in range(2):
            b_, o = buf(o, [512])
            sl.append(b_)
        coef, o = buf(o, [NT, 32])
        assert o <= F0, o
        o = B0
        ln2g, o = buf(o, [D])
        ln2b, o = buf(o, [D])
        x2rb = []
        a2 = []
        for i in range(4):
            b_, o = buf(o, [D])
            x2rb.append(b_)
        for i in range(2):
            b_, o = buf(o, [D])
            a2.append(b_)
        ln2s = []
        for i in range(2):
            stats2, o = buf(o, [2, 6])
            mv2, o = buf(o, [2])
            rstd2, o = buf(o, [1])
            nbias2, o = buf(o, [1])
            ln2s.append((stats2, mv2, rstd2, nbias2))
        assert o <= C0

        if dbg:
            def dout(name, shape, dt):
                return nc.dram_tensor(name, list(shape), dt, kind="ExternalOutput").ap()
            if 1 <= stop <= 4:
                dbg_list.append((dout("dbg_mixT", [128, 4, S_LEN], BF16), mixT.v()))
            if 2 <= stop <= 3:
                dbg_list.append((dout("dbg_qT", [128, 4, S_LEN], BF16), qT.v()))
                dbg_list.append((dout("dbg_kT", [128, S_LEN], BF16), kT.v()))
                dbg_list.append((dout("dbg_vaug", [128, NT, 2, 65], BF16), vaug.v()))
            if 3 <= stop <= 4:
                dbg_list.append((dout("dbg_oT", [128, 4, S_LEN], BF16), oT.v()))
            if stop >= 4:
                dbg_list.append((dout("dbg_coef", [128, NT, 32], F32), coef.v()))
            if stop == 4:
                dbg_list.append((dout("dbg_x2T", [128, 8, S_LEN], BF16), xT.v()))
            if stop >= 4.5:
                dbg_list.append((dout("dbg_d1", [128, NT], F32), d1f.v()))
                dbg_list.append((dout("dbg_d2", [128, NT], F32), d2f.v()))
                dbg_list.append((dout("dbg_es", [128, N_SLOTS], F32), es_f.v()))
                dbg_list.append((dout("dbg_w12", [128, 2, NT], F32), w12.v()))

        def D_(ap, name="dram_in", lo=0, hi=1):
            return View(ap, name, [(lo, hi)])

        def body():
            dma("sp", b_in.v(), D_(d_bin))
            dma("sp", bmix.v(), D_(d_bmix))
            dma("sp", pscale.v(), D_(d_pscale))
            dma("sp", esink.v(), D_(d_sink))
            dma("sp", br_bc.v(), D_(d_br))
            dma("sp", ec.v(), D_(d_ec))
            dma("sp", wr_f.v(), D_(d_wr))
            dma("sp", bv_bc.v(), D_(d_bv))
            dma("sp", cosT.v(), D_(d_cos))
            dma("sp", sinT.v(), D_(d_sin))
            dma("pool", wmix.v(), D_(d_wmix))
            for k in range(8):
                dma("pool", xT.v(k), D_(d_xT[k * 128:(k + 1) * 128, :]))
            memset("pool", ident_f.v(), 0.0)
            S.op("pool", lambda e: e.affine_select(out=ident_f.v().ap, in_=ident_f.v().ap, pattern=[[-1, 128]],
                                                   compare_op=ALU.not_equal, fill=1.0, base=0, channel_multiplier=1),
                 reads=[ident_f.v()], writes=[ident_f.v()])
            cp("dve", ident_b.v(), ident_f.v())
            tt("dve", bms.v(), bmix.v(), pscale.v(), ALU.mult)
            act(esink.v(), esink.v(), AF.Exp)
            memset("pool", vaug.v(), 1.0)
            memset("dve", uT.v(), 0.0)

            wch_seq = ([0, 1, 2, 3, 4, 8, 5, 9, 6, 10, 7, 11, 12, 13, 14]
                       + [c for _n in range(NS) for m_ in range(8) for c in (15 + m_, 23 + m_)])
            wch_pos = [0]
            wch_issued = [0]
            WCH_AHEAD = 2

            def load_wchunk(ch):
                pos = wch_pos[0]
                assert wch_seq[pos] == ch, (pos, ch)
                while wch_issued[0] < min(len(wch_seq), pos + 1 + WCH_AHEAD):
                    j = wch_issued[0]
                    dma("pool", wch[j % 4].v(), D_(d_win[wch_seq[j]]))
                    wch_issued[0] += 1
                wch_pos[0] += 1
                return wch[pos % 4]

            def inproj_fm(ch):
                w = load_wchunk(ch)
                bks = [next_bank() for _ in range(NS)]
                for k in range(8):
                    for n in range(NS):
                        mm(PSF[bks[n]].v(), w.v(k), xT.v(k, slice(n * 512, (n + 1) * 512)), k == 0, k == 7)
                return bks

            for g in range(4):
                wdw = 2 ** (g + 1)
                bks = inproj_fm(g)
                for n in range(NS):
                    act(uT.v(slice(8 + n * 512, 8 + (n + 1) * 512)), PSF[bks[n]].v(), AF.Identity,
                        bias=b_in.v(slice(g, g + 1)))
                W_ = PADW
                src = uT
                lvl = [(1, sA), (2, sB), (4, sA), (8, sB)]
                tt("dve", sA.v(slice(1, W_)), uT.v(slice(0, W_ - 1)), uT.v(slice(1, W_)), ALU.add)
                cur = sA
                lo, hi = 1, W_
                half = 1
                for li in range(1, g + 1):
                    dst = sB if cur is sA else sA
                    nlo, nhi = lo + half, hi - half
                    tt("dve", dst.v(slice(nlo, nhi)), cur.v(slice(nlo - half, nhi - half)),
                       cur.v(slice(nlo + half, nhi + half)), ALU.add)
                    cur = dst
                    lo, hi = nlo, nhi
                    half *= 2
                assert lo <= 8 and hi >= S_LEN + 8
                ts("dve", cur.v(slice(8, 8 + S_LEN)), cur.v(slice(8, 8 + S_LEN)), 1.0 / wdw, ALU.mult)
                tt("dve", cur.v(slice(8, 16)), cur.v(slice(8, 16)), ec.v(g, slice(0, 8)), ALU.mult)
                tt("dve", cur.v(slice(S_LEN, S_LEN + 8)), cur.v(slice(S_LEN, S_LEN + 8)), ec.v(g, slice(8, 16)), ALU.mult)
                tt("dve", pooled.v(), cur.v(slice(8, 8 + S_LEN)), uT.v(slice(8, 8 + S_LEN)), ALU.subtract)
                for n in range(NS):
                    b = next_bank()
                    mm(PSF[b].v(), wmix.v(g), pooled.v(slice(n * 512, (n + 1) * 512)), True, True)
                    act(mixT.v(g, slice(n * 512, (n + 1) * 512)), PSF[b].v(), AF.Identity,
                        bias=bms.v(slice(g, g + 1)), scale=pscale.v(slice(g, g + 1)))

            if stop <= 1:
                return
            def rope_chunk(ch_main, ch_swap, dst_fn):
                bk_m = inproj_fm(ch_main)
                bk_s = inproj_fm(ch_swap)
                for n in range(NS):
                    cs = slice(n * 512, (n + 1) * 512)
                    stt(rtmp1.v(), PSF[bk_m[n]].v(), b_in.v(slice(ch_main, ch_main + 1)), cosT.v(cs), ALU.add, ALU.mult)
                    stt(rtmp2.v(), PSF[bk_s[n]].v(), b_in.v(slice(ch_swap, ch_swap + 1)), sinT.v(cs), ALU.add, ALU.mult)
                    tt("pool", dst_fn(cs), rtmp1.v(), rtmp2.v(), ALU.add)

            for c in range(4):
                rope_chunk(4 + c, 8 + c, lambda cs, c=c: qT.v(c, cs))
            rope_chunk(12, 13, lambda cs: kT.v(cs))
            wv = load_wchunk(14)
            for t in range(NT):
                b = next_bank()
                pv = PSF[b]
                for k in range(8):
                    mm(pv.v(slice(0, 128)), xT.v(k, slice(t * 128, (t + 1) * 128)), wv.v(k), k == 0, k == 7)
                for h in range(2):
                    tt("dve", vaug.v(t, h, slice(0, 64)), pv.v(slice(h * 64, (h + 1) * 64)),
                       bv_bc.v(slice(h * 64, (h + 1) * 64)), ALU.add)

            if stop <= 2:
                return
            for i in range(NT):
                ot = otok[i % 2]
                qs = slice(i * 128, (i + 1) * 128)
                for g in range(2):
                    pg = slice(64 * g, 64 * g + 64)
                    pt = pT[(2 * i + g) % 2]
                    js = [j for j in (i - 1, i, i + 1) if 0 <= j < NT]
                    for jj, j in enumerate(js):
                        b = next_bank()
                        for hh in range(4):
                            mm(PSF[b].v(slice(hh * 128, (hh + 1) * 128)),
                               kT.v(slice(j * 128, (j + 1) * 128), p=pg), qT.v(hh, qs, p=pg), True, True)
                        act(pt.v(jj), PSF[b].v(), AF.Exp, scale=0.125)
                        if j == i - 1:
                            S.op("pool", lambda e, v=pt.v(jj): e.affine_select(
                                out=v.ap, in_=v.ap, pattern=[[0, 4], [-1, 128]], compare_op=ALU.is_ge,
                                fill=0.0, base=0, channel_multiplier=1), reads=[pt.v(jj)], writes=[pt.v(jj)])
                        elif j == i + 1:
                            S.op("pool", lambda e, v=pt.v(jj): e.affine_select(
                                out=v.ap, in_=v.ap, pattern=[[0, 4], [1, 128]], compare_op=ALU.is_ge,
                                fill=0.0, base=0, channel_multiplier=-1), reads=[pt.v(jj)], writes=[pt.v(jj)])
                    bo = next_bank()
                    po = psbuf(bo, [4, 65])
                    for hh in range(4):
                        for jj, j in enumerate(js):
                            mm(po.v(hh), pt.v(jj, slice(hh * 128, (hh + 1) * 128)), vaug.v(j, g),
                               jj == 0, jj == len(js) - 1)
                    tt("dve", den.v(), po.v(slice(0, 4), 64), esink.v(slice(4 * g, 4 * g + 4)), ALU.add)
                    S.op("dve", lambda e: e.reciprocal(out=rden.v().ap, in_=den.v().ap), reads=[den.v()], writes=[rden.v()])
                    for hh in range(4):
                        hd = 4 * g + hh
                        ts("dve", ot.v(slice(hd * 64, (hd + 1) * 64)), po.v(hh, slice(0, 64)),
                           rden.v(slice(hh, hh + 1)), ALU.mult)
                bt = next_bank()
                ptb = psbuf(bt, [4, 256], BF16)
                for c in range(4):
                    tr(ptb.v(c, slice(0, 128)), ot.v(slice(c * 128, (c + 1) * 128)), ident_b.v())
                cp("act", oT.v(slice(0, 4), qs), ptb.v(slice(0, 4), slice(0, 128)))

            if stop <= 3:
                return
            dma("pool", wpb.v(), D_(d_wpb))
            dma("pool", wab.v(), D_(d_wab))
            dma("pool", wo.v(), D_(d_wo))
            dma("sp", ln1g.v(), D_(d_ln1g))
            dma("sp", ln1b.v(), D_(d_ln1b))

            def layer_norm(a, stats_b, mv_b, rstd_b, g_b, b_b, out, mul_eng="pool", nbias_b=None):
                for c in range(2):
                    S.op("dve", lambda e, c=c: e.bn_stats(out=stats_b.v(c).ap, in_=a.v(slice(c * 512, (c + 1) * 512)).ap),
                         reads=[a.v(slice(c * 512, (c + 1) * 512))], writes=[stats_b.v(c)])
                S.op("dve", lambda e: e.bn_aggr(out=mv_b.v().ap, in_=stats_b.v().ap), reads=[stats_b.v()], writes=[mv_b.v()])
                act(rstd_b.v(), mv_b.v(slice(1, 2)), AF.Sqrt, bias=LN_EPS)
                S.op("dve", lambda e: e.reciprocal(out=rstd_b.v().ap, in_=rstd_b.v().ap), reads=[rstd_b.v()], writes=[rstd_b.v()])
                if nbias_b is None:
                    ts("dve", a.v(), a.v(), mv_b.v(slice(0, 1)), ALU.subtract, rstd_b.v(), ALU.mult)
                else:
                    ts("dve", nbias_b.v(), mv_b.v(slice(0, 1)), rstd_b.v(), ALU.mult, -1.0, ALU.mult)
                    act(a.v(), a.v(), AF.Identity, bias=nbias_b.v(), scale=rstd_b.v())
                tt(mul_eng, a.v(), a.v(), g_b.v(), ALU.mult)
                tt("dve", out.v(), a.v(), b_b.v(), ALU.add)

            def stage_A(n):
                cs = slice(n * 512, (n + 1) * 512)
                for m in range(8):
                    ms = slice(m * 128, (m + 1) * 128)
                    w0 = load_wchunk(15 + m)
                    w1 = load_wchunk(23 + m)
                    byp, bya, bg0, bg1 = next_bank(), next_bank(), next_bank(), next_bank()
                    for k in range(4):
                        mm(PSF[byp].v(), wpb.v(k, ms), mixT.v(k, cs), k == 0, k == 3)
                    for k in range(4):
                        mm(PSF[bya].v(), wab.v(k, ms), oT.v(k, cs), k == 0, k == 3)
                    for k in range(8):
                        mm(PSF[bg0].v(), w0.v(k), xT.v(k, cs), k == 0, k == 7)
                    for k in range(8):
                        mm(PSF[bg1].v(), w1.v(k), xT.v(k, cs), k == 0, k == 7)
                    act(sg0.v(), PSF[bg0].v(), AF.Sigmoid, bias=b_in.v(slice(15 + m, 16 + m)))
                    act(sg1.v(), PSF[bg1].v(), AF.Sigmoid, bias=b_in.v(slice(23 + m, 24 + m)))
                    tt("dve", t0b.v(), PSF[byp].v(), sg0.v(), ALU.mult)
                    tt("dve", t1b.v(), PSF[bya].v(), sg1.v(), ALU.mult)
                    tt("pool", mT.v(m), t0b.v(), t1b.v(), ALU.add)

            def stage_B(n):
                for tl in range(4):
                    t = n * 4 + tl
                    dma("sp", xtok[t % 4].v(), D_(d_x[t * 128:(t + 1) * 128, :]))
                for tl in range(4):
                    t = n * 4 + tl
                    xt = xtok[t % 4]
                    ab = abuf[t % 2]
                    for hf in range(2):
                        b = next_bank()
                        for m in range(8):
                            mm(PSF[b].v(), mT.v(m, slice(tl * 128, (tl + 1) * 128)), wo.v(m, slice(hf * 512, (hf + 1) * 512)),
                               m == 0, m == 7)
                        stt(ab.v(slice(hf * 512, (hf + 1) * 512)), xt.v(slice(hf * 512, (hf + 1) * 512)), ALPHA,
                            PSF[b].v(), ALU.mult, ALU.add)
                    layer_norm(ab, stats, mv, rstd, ln1g, ln1b, xt)
                    dma("sp", View(d_x2s[t * 128:(t + 1) * 128, :], "x2s", [(t, t + 1)]), xt.v())

            def stage_C(n):
                for tl in range(4):
                    t = n * 4 + tl
                    xt = xtok[t % 4]
                    b0, b1 = next_bank(), next_bank()
                    for k in range(8):
                        bb = b0 if k < 4 else b1
                        kk = k % 4
                        tr(PSF[bb].v(slice(kk * 128, (kk + 1) * 128)), xt.v(slice(k * 128, (k + 1) * 128)), ident_f.v())
                    for hb, bb in enumerate((b0, b1)):
                        pview = psbuf(bb, [4, 128])
                        cp("act", x2Tf.v(slice(hb * 4, hb * 4 + 4)), pview.v())
                    br_ = next_bank()
                    for k in range(8):
                        mm(PSF[br_].v(slice(0, 36)), x2Tf.v(k), wr_f.v(k), k == 0, k == 7)
                    tt("dve", lg.v(), PSF[br_].v(slice(0, 36)), br_bc.v(), ALU.add)
                    R = lambda a, b=None: rt.v(slice(a, (a + 1) if b is None else b))
                    S.op("dve", lambda e: e.tensor_reduce(out=R(0).ap, in_=lg.v(slice(0, 4)).ap, axis=AX.X, op=ALU.max, negate=True),
                         reads=[lg.v(slice(0, 4))], writes=[R(0)])
                    act(R(7, 11), lg.v(slice(0, 4)), AF.Exp, bias=R(0), accum=R(1))
                    S.op("dve", lambda e: e.reciprocal(out=R(2).ap, in_=R(1).ap), reads=[R(1)], writes=[R(2)])
                    ts("dve", R(3, 7), lg.v(slice(0, 4)), R(0), ALU.add, 0.0, ALU.is_ge)
                    ts("dve", R(11, 19), lg.v(slice(4, 12)), R(3), ALU.mult)
                    for gg in range(1, 4):
                        stt(R(11, 19), lg.v(slice(4 + 8 * gg, 12 + 8 * gg)), R(3 + gg), R(11, 19), ALU.mult, ALU.add)
                    S.op("dve", lambda e: e.tensor_reduce(out=R(19).ap, in_=R(11, 19).ap, axis=AX.X, op=ALU.max),
                         reads=[R(11, 19)], writes=[R(19)])
                    ts("dve", R(20, 28), R(11, 19), R(19), ALU.is_ge)
                    stt(R(28, 36), R(20, 28), -1e30, R(11, 19), ALU.mult, ALU.add)
                    S.op("dve", lambda e: e.tensor_reduce(out=R(36).ap, in_=R(28, 36).ap, axis=AX.X, op=ALU.max),
                         reads=[R(28, 36)], writes=[R(36)])
                    ts("dve", R(37, 45), R(28, 36), R(36), ALU.is_ge)
                    tt("dve", R(45), R(36), R(19), ALU.subtract)
                    act(R(46), R(45), AF.Exp)
                    ts("dve", R(47), R(46), 1.0, ALU.add)
                    S.op("dve", lambda e: e.reciprocal(out=R(47).ap, in_=R(47).ap), reads=[R(47)], writes=[R(47)])
                    tt("dve", R(48), R(47), R(2), ALU.mult)
                    tt("dve", R(49), R(48), R(46), ALU.mult)
                    ts("dve", R(50, 58), R(20, 28), R(48), ALU.mult)
                    stt(R(50, 58), R(37, 45), R(49), R(50, 58), ALU.mult, ALU.add)
                    for gg in range(4):
                        ts("dve", coef.v(t, slice(8 * gg, 8 * gg + 8)), R(50, 58), R(3 + gg), ALU.mult)
                        ts("dve", OH1f.v(t, slice(8 * gg, 8 * gg + 8)), R(20, 28), R(3 + gg), ALU.mult)
                        ts("dve", OH2f.v(t, slice(8 * gg, 8 * gg + 8)), R(37, 45), R(3 + gg), ALU.mult)
                    cp("dve", w12.v(0, slice(t, t + 1)), R(48))
                    cp("dve", w12.v(1, slice(t, t + 1)), R(49))

            stage_A(0)
            for n in range(NS):
                stage_B(n)
                if n + 1 < NS:
                    stage_A(n + 1)
                stage_C(n)

            if stop <= 4.5:
                return
            def load_w_static(s_):
                wg_b, wu_b, wd_b = wexp[s_ % NWS]
                dma("pool", wg_b.v(), D_(d_wg[s_]))
                dma("pool", wu_b.v(), D_(d_wu[s_]))
                dma("pool", wd_b.v(), D_(d_wd[s_]))
            if stop >= 5:
                for s_ in range(NWS):
                    load_w_static(s_)
            tt("dve", ind.v(), OH1f.v(), OH2f.v(), ALU.add)
            memset("pool", ones_bf.v(), 1.0)
            memset("pool", stri_bf.v(), 1.0)
            S.op("pool", lambda e: e.affine_select(out=stri_bf.v().ap, in_=stri_bf.v().ap, pattern=[[1, 128]],
                                                   compare_op=ALU.is_ge, fill=0.0, base=-1, channel_multiplier=-1),
                 reads=[stri_bf.v()], writes=[stri_bf.v()])
            memset("dve", zeros32.v(), 0.0)
            S.op("pool", lambda e: e.iota(pidx_i.v().ap, [[0, 1]], base=0, channel_multiplier=1), writes=[pidx_i.v()])
            cp("dve", pidx.v(), pidx_i.v())
            pb, cb = next_bank(), next_bank()
            for i in range(NT):
                for j in range(i + 1):
                    mm(PSF[pb].v(slice(i * 32, (i + 1) * 32)), (ones_bf if j < i else stri_bf).v(), ind.v(j), j == 0, j == i)
            for j in range(NT):
                mm(PSF[cb].v(slice(0, 32)), ones_bf.v(), ind.v(j), j == 0, j == NT - 1)
            cp("dve", prefix.v(), psbuf(pb, [NT, 32]).v())
            cp("dve", cnt.v(), PSF[cb].v(slice(0, 32)))
            ts("dve", cm_f.v(), cnt.v(), -float(SLOT_B), ALU.add, 0.0, ALU.max)
            cp("dve", cnt_i.v(), cm_f.v())
            ts("dve", nb_i.v(), cnt_i.v(), SLOT_B - 1, ALU.add)
            ts("dve", nb_i.v(), nb_i.v(), SLOT_SHIFT, ALU.arith_shift_right)
            cp("dve", nb_f.v(), nb_i.v())
            S.op("dve", lambda e: e.tensor_tensor_scan(out=pendb.v().ap, data0=zeros32.v().ap, data1=nb_f.v().ap,
                                                       initial=0.0, op0=ALU.add, op1=ALU.add),
                 reads=[zeros32.v(), nb_f.v()], writes=[pendb.v()])
            S.op("pool", lambda e: e.iota(basep_i.v().ap, [[SLOT_B, N_EXP]], base=0, channel_multiplier=0),
                 writes=[basep_i.v()])
            cp("dve", basep.v(), basep_i.v())
            tt("dve", pstart.v(), pendb.v(), nb_f.v(), ALU.subtract)
            ts("dve", pstart.v(), pstart.v(), float(SLOT_B), ALU.mult, float(N_EXP * SLOT_B - SLOT_B), ALU.add)
            tt("dve", pstart.v(), pstart.v(), basep.v(), ALU.subtract)
            for s_ in range(N_OVF):
                S.op("dve", lambda e, s_=s_: e.tensor_scalar(out=junk32.v().ap, in0=pendb.v().ap, scalar1=float(s_), scalar2=0.0,
                                                             op0=ALU.is_le, op1=ALU.add, accum_out=es_f.v(slice(s_, s_ + 1)).ap),
                     reads=[pendb.v()], writes=[junk32.v(), es_f.v(slice(s_, s_ + 1))])
            ts("dve", widx_f.v(slice(0, N_OVF)), es_f.v(slice(0, N_OVF)), 128.0, ALU.mult, pidx.v(), ALU.add)
            cp("dve", widx_i.v(slice(0, N_OVF)), widx_f.v(slice(0, N_OVF)))
            for t in range(NT):
                ts("dve", slotpos.v(t), prefix.v(t), float(SLOT_B), ALU.is_ge)
                tt("dve", slotpos.v(t), slotpos.v(t), pstart.v(), ALU.mult)
                tt("dve", slotpos.v(t), slotpos.v(t), prefix.v(t), ALU.add)
                tt("dve", slotpos.v(t), slotpos.v(t), basep.v(), ALU.add)
            for (ohf, dfl, din_) in ((OH1f, d1f, d1i), (OH2f, d2f, d2i)):
                tt("dve", tmp512.v(), ohf.v(), slotpos.v(), ALU.mult)
                S.op("dve", lambda e, dfl=dfl: e.tensor_reduce(out=dfl.v().ap, in_=tmp512.v().ap, axis=AX.X, op=ALU.add),
                     reads=[tmp512.v()], writes=[dfl.v()])
                cp("dve", din_.v(), dfl.v())
            if stop <= 4.8:
                return
            for t in range(NT):
                xr_ = xr[t % 4]
                dma("sp", xr_.v(), View(d_x2s[t * 128:(t + 1) * 128, :], "x2s", [(t, t + 1)]))
                for j, din_ in enumerate((d1i, d2i)):
                    S.op("pool", lambda e, xr_=xr_, din_=din_, t=t: e.indirect_dma_start(
                        out=d_xin[:, :], out_offset=bass.IndirectOffsetOnAxis(ap=din_.v(slice(t, t + 1)).ap, axis=0),
                        in_=xr_.v().ap, in_offset=None),
                        reads=[xr_.v(), din_.v(slice(t, t + 1))], writes=[View(None, "xin", [(2 * t + j, 2 * t + j + 1)])], dma=True)
            if stop <= 4.9:
                return
            _bc = {}

            def _bc_reg(e):
                if "r" not in _bc:
                    _bc["r"] = e.to_reg(N_EXP * 128 - 1)
                return _bc["r"]
            xin_all = View(None, "xin", [(0, 2 * NT)])
            NQ = N_SLOTS * SUBS

            order = list(range(NWS))
            _ip, _io = NWS, N_EXP
            for ch_ in "PPOPPOPPOPO" * 4:
                if ch_ == "P":
                    order.append(_ip)
                    _ip += 1
                else:
                    order.append(_io)
                    _io += 1
            assert sorted(order) == list(range(N_SLOTS)), order

            def load_w(pos_):
                s_ = order[pos_]
                if s_ < N_EXP:
                    wg_b, wu_b, wd_b = wexp[pos_ % NWS]
                    dma("pool", wg_b.v(), D_(d_wg[s_]))
                    dma("pool", wu_b.v(), D_(d_wu[s_]))
                    dma("pool", wd_b.v(), D_(d_wd[s_]))
                    return
                wg2, wu2, wd2 = wexp2[pos_ % NWS]
                s_ = s_ - N_EXP
                for (w2_, drows) in ((wg2, d_wg_rows), (wu2, d_wu_rows), (wd2, d_wd_rows)):
                    S.op("pool", lambda e, w2_=w2_, drows=drows, s_=s_: e.indirect_dma_start(
                        out=w2_.v().ap, out_offset=None, in_=drows,
                        in_offset=bass.IndirectOffsetOnAxis(ap=widx_i.v(slice(s_, s_ + 1)).ap, axis=0),
                        bounds_check=_bc_reg(e), oob_is_err=False),
                        reads=[widx_i.v(slice(s_, s_ + 1))], writes=[w2_.v()], dma=True)

            def stage_L(qs):
                q = order[qs // SUBS] * SUBS + qs % SUBS
                r0 = q * 128
                xb_ = xb[qs % 4]
                S.op("act", lambda e, xb_=xb_, r0=r0: e.dma_start(out=xb_.v().ap, in_=d_xin[r0:r0 + 128, :]),
                     reads=[xin_all], writes=[xb_.v()], dma=True)

            def stage_T(qs):
                xb_ = xb[qs % 4]
                xbT_ = xbT[qs % 4]
                for hb in range(2):
                    bt = next_bank()
                    ptb = psbuf(bt, [4, 256], BF16)
                    for kk in range(4):
                        k = hb * 4 + kk
                        tr(ptb.v(kk, slice(0, 128)), xb_.v(slice(k * 128, (k + 1) * 128)), ident_b.v())
                    cp("act" if hb == 0 else "dve", xbT_.v(slice(hb * 4, hb * 4 + 4)), ptb.v(slice(0, 4), slice(0, 128)))

            def stage_G(qs):
                wg_b, wu_b, wd_b = wexp[(qs // SUBS) % NWS]
                xbT_ = xbT[qs % 4]
                hT_ = hTs[qs % 4]
                bg, bu = next_bank(), next_bank()
                for f in range(4):
                    fs = slice(f * 128, (f + 1) * 128)
                    for k in range(8):
                        mm(PSF[bg].v(fs), wg_b.v(k, fs), xbT_.v(k), k == 0, k == 7)
                for f in range(4):
                    fs = slice(f * 128, (f + 1) * 128)
                    for k in range(8):
                        mm(PSF[bu].v(fs), wu_b.v(k, fs), xbT_.v(k), k == 0, k == 7)
                s_l = sl[qs % 2]
                act(s_l.v(), PSF[bg].v(), AF.Silu)
                tt("dve", hT_.v(), PSF[bu].v(), s_l.v(), ALU.mult)

            def stage_D(qs):
                q = order[qs // SUBS] * SUBS + qs % SUBS
                wg_b, wu_b, wd_b = wexp[(qs // SUBS) % NWS]
                hT4 = hTs4[qs % 4]
                yb_ = ybs[qs % 4]
                r0 = q * 128
                for hf in range(2):
                    b = next_bank()
                    for f in range(4):
                        mm(PSF[b].v(), hT4.v(f), wd_b.v(f, slice(hf * 512, (hf + 1) * 512)), f == 0, f == 3)
                    cp("act" if hf == 0 else "dve", yb_.v(slice(hf * 512, (hf + 1) * 512)), PSF[b].v())
                S.op("sp", lambda e, yb_=yb_, r0=r0: e.dma_start(out=d_yb[r0:r0 + 128, :], in_=yb_.v().ap),
                     reads=[yb_.v()], writes=[View(None, "yb", [(q, q + 1)])], dma=True)

            next_w = NWS
            XB_AHEAD = 2
            for it in range(NQ + 2):
                while next_w < N_SLOTS and (next_w < NWS or it >= (next_w - NWS) * SUBS + SUBS + 2):
                    load_w(next_w)
                    next_w += 1
                if it == 0:
                    for q_ in range(min(XB_AHEAD, NQ)):
                        stage_L(q_)
                if it + XB_AHEAD < NQ:
                    stage_L(it + XB_AHEAD)
                if it < NQ:
                    stage_T(it)
                if it >= 2:
                    stage_D(it - 2)
                if 1 <= it <= NQ:
                    stage_G(it - 1)
            assert next_w == N_SLOTS

            if stop <= 5:
                return
            yb_all = View(None, "yb", [(0, N_SLOTS * SUBS)])
            dma("sp", ln2g.v(), D_(d_ln2g))
            dma("sp", ln2b.v(), D_(d_ln2b))
            X2_AHEAD = 3

            def load_x2r(t):
                S.op("act", lambda e, t=t: e.dma_start(out=x2rb[t % 4].v().ap, in_=d_x2s[t * 128:(t + 1) * 128, :]),
                     reads=[View(None, "x2s", [(t, t + 1)])], writes=[x2rb[t % 4].v()], dma=True)
            for t in range(min(X2_AHEAD, NT)):
                load_x2r(t)
            def tail_front(t):
                xr_ = x2rb[t % 4]
                ab = a2[t % 2]
                g1, g2 = rg1[t % 2], rg2[t % 2]
                stats_b, mv_b, rstd_b, nbias_b = ln2s[t % 2]
                if t + X2_AHEAD < NT:
                    load_x2r(t + X2_AHEAD)
                for (gb, din_) in ((g1, d1i), (g2, d2i)):
                    S.op("pool", lambda e, gb=gb, din_=din_, t=t: e.indirect_dma_start(
                        out=gb.v().ap, out_offset=None, in_=d_yb[:, :],
                        in_offset=bass.IndirectOffsetOnAxis(ap=din_.v(slice(t, t + 1)).ap, axis=0)),
                        reads=[yb_all, din_.v(slice(t, t + 1))], writes=[gb.v()], dma=True)
                act(g1.v(), g1.v(), AF.Identity, scale=w12.v(0, slice(t, t + 1)))
                stt(g1.v(), g2.v(), w12.v(1, slice(t, t + 1)), g1.v(), ALU.mult, ALU.add)
                stt(ab.v(), xr_.v(), ALPHA, g1.v(), ALU.mult, ALU.add)
                for c in range(2):
                    S.op("dve", lambda e, c=c: e.bn_stats(out=stats_b.v(c).ap, in_=ab.v(slice(c * 512, (c + 1) * 512)).ap),
                         reads=[ab.v(slice(c * 512, (c + 1) * 512))], writes=[stats_b.v(c)])
                S.op("dve", lambda e: e.bn_aggr(out=mv_b.v().ap, in_=stats_b.v().ap), reads=[stats_b.v()], writes=[mv_b.v()])
                act(rstd_b.v(), mv_b.v(slice(1, 2)), AF.Sqrt, bias=LN_EPS)
                S.op("dve", lambda e: e.reciprocal(out=rstd_b.v().ap, in_=rstd_b.v().ap), reads=[rstd_b.v()], writes=[rstd_b.v()])
                ts("dve", nbias_b.v(), mv_b.v(slice(0, 1)), rstd_b.v(), ALU.mult, -1.0, ALU.mult)
                act(ab.v(), ab.v(), AF.Identity, bias=nbias_b.v(), scale=rstd_b.v())

            def tail_back(t):
                ab = a2[t % 2]
                tt("dve", ab.v(), ab.v(), ln2g.v(), ALU.mult)
                tt("dve", ab.v(), ab.v(), ln2b.v(), ALU.add)
                dma("sp", View(d_out[t * 128:(t + 1) * 128, :], "out", [(t, t + 1)]), ab.v())

            tail_front(0)
            for t in range(NT):
                if t + 1 < NT:
                    tail_front(t + 1)
                tail_back(t)

        body()
        for (dv, bv_) in dbg_list:
            dma("sp", View(dv, "dbgout", [(0, 1)]), bv_)
        S.emit(es)
    return nc


def _prep_shared(inp, with_experts=True):
    f = np.float32
    w_in = np.asarray(inp["w_in"], f)[0]
    b_in = np.asarray(inp["b_in"], f)[0]
    cols = []
    for g in range(4):
        cols.append(np.arange(g * 128, (g + 1) * 128))
    d = np.arange(64)
    sw = (d + 32) % 64
    for c in range(4):
        cols.append(np.concatenate([512 + c * 64 + d, 512 + (4 + c) * 64 + d]))
    for c in range(4):
        cols.append(np.concatenate([512 + c * 64 + sw, 512 + (4 + c) * 64 + sw]))
    cols.append(np.concatenate([1024 + d, 1024 + 64 + d]))
    cols.append(np.concatenate([1024 + sw, 1024 + 64 + sw]))
    cols.append(np.arange(1152, 1280))
    for m in range(16):
        cols.append(np.arange(1280 + m * 128, 1280 + (m + 1) * 128))
    cols = np.stack(cols)
    assert cols.shape == (N_INCH, 128)
    wsel = w_in[:, cols]
    w_in_r = np.ascontiguousarray(wsel.reshape(8, 128, N_INCH, 128).transpose(2, 1, 0, 3))
    b_in_r = np.ascontiguousarray(b_in[cols].T)
    bv_bc = np.ascontiguousarray(np.broadcast_to(b_in[1152:1280][None, :], (128, 128)))
    pos = np.arange(S_LEN, dtype=f)
    inv = (f(10000.0) ** (-np.arange(0, 64, 2, dtype=f) / f(64))).astype(f)
    ang = (pos[:, None] * inv[None, :]).astype(f)
    cos = np.cos(ang).astype(f)
    sin = np.sin(ang).astype(f)
    p = np.arange(128)
    fi = (p % 64) % 32
    sign = np.where((p % 64) < 32, -1.0, 1.0).astype(f)
    cosT = np.ascontiguousarray(cos[:, fi].T)
    sinT = np.ascontiguousarray((sin[:, fi] * sign[None, :]).T.astype(f))
    ec = np.ones((128, 4, 16), f)
    tpos = np.concatenate([np.arange(8), np.arange(S_LEN - 8, S_LEN)])
    for g, w in enumerate((2, 4, 8, 16)):
        lo = np.clip(tpos - w // 2, 0, S_LEN)
        hi = np.clip(tpos - w // 2 + w, 0, S_LEN)
        ec[:, g, :] = (f(w) / (hi - lo).astype(f))[None, :]
    rep = lambda v, n=128: np.ascontiguousarray(np.broadcast_to(np.asarray(v, f)[None, :], (n, len(v))))
    sh = {
        "w_in_r": w_in_r, "b_in_r": b_in_r, "bv_bc": bv_bc, "cosT": cosT, "sinT": sinT, "ec": ec,
        "wmix_r": np.ascontiguousarray(np.asarray(inp["w_pool_mix"], f)[0].transpose(1, 0, 2)),
        "bmix_r": np.ascontiguousarray(np.asarray(inp["b_pool_mix"], f)[0].T),
        "pscale_r": np.ascontiguousarray(np.asarray(inp["pool_scale"], f)[0].reshape(4, 128).T),
        "sink_bc": rep(np.asarray(inp["attn_sink"], f)[0]),
        "wpb_r": np.ascontiguousarray(np.asarray(inp["w_pool_br"], f)[0].reshape(4, 128, D).transpose(1, 0, 2)),
        "wab_r": np.ascontiguousarray(np.asarray(inp["w_attn_br"], f)[0].reshape(4, 128, D).transpose(1, 0, 2)),
        "wo_r": np.ascontiguousarray(np.asarray(inp["w_o"], f)[0].reshape(8, 128, D).transpose(1, 0, 2)),
        "ln1g_bc": rep(np.asarray(inp["ln1_g"], f)[0]), "ln1b_bc": rep(np.asarray(inp["ln1_b"], f)[0]),
        "ln2g_bc": rep(np.asarray(inp["ln2_g"], f)[0]), "ln2b_bc": rep(np.asarray(inp["ln2_b"], f)[0]),
        "wr_r": np.ascontiguousarray(np.concatenate([np.asarray(inp["w_router_group"], f)[0],
                                                     np.asarray(inp["w_router_expert"], f)[0]], axis=1)
                                     .reshape(8, 128, 36).transpose(1, 0, 2)),
        "br_bc": rep(np.concatenate([np.asarray(inp["b_router_group"], f)[0], np.asarray(inp["b_router_expert"], f)[0]])),
    }
    if with_experts:
        sh.update({
        "wg_r": np.ascontiguousarray(np.asarray(inp["w_gate"], f)[0].reshape(N_EXP, 8, 128, 512).transpose(0, 2, 1, 3)),
        "wu_r": np.ascontiguousarray(np.asarray(inp["w_up"], f)[0].reshape(N_EXP, 8, 128, 512).transpose(0, 2, 1, 3)),
        "wd_r": np.ascontiguousarray(np.asarray(inp["w_down"], f)[0].reshape(N_EXP, 4, 128, D).transpose(0, 2, 1, 3)),
        })
    return sh


_NC_CACHE = {}


def run_debug(inputs, stop, n_cores=N_CORES):
    x = np.asarray(inputs["x"], np.float32)
    sh = _prep_shared(inputs, with_experts=(stop >= 5))
    nc = build_program(stop=stop, dbg=True)
    in_maps = []
    for c in range(n_cores):
        m = dict(sh)
        m["x_tok"] = np.ascontiguousarray(x[c])
        m["xT"] = np.ascontiguousarray(x[c].T)
        in_maps.append(m)
    res = run_bass_kernel_spmd(nc, in_maps, core_ids=list(range(n_cores)))
    return res.results


def kernel(**inputs):
    x = np.asarray(inputs["x"], np.float32)
    sh = _prep_shared(inputs)
    if "nc" not in _NC_CACHE:
        _NC_CACHE["nc"] = build_program()
    nc = _NC_CACHE["nc"]
    in_maps = []
    for c in range(N_CORES):
        m = dict(sh)
        m["x_tok"] = np.ascontiguousarray(x[c])
        m["xT"] = np.ascontiguousarray(x[c].T)
        in_maps.append(m)
    res = run_bass_kernel_spmd(nc, in_maps, core_ids=list(range(N_CORES)))
    out = np.stack([np.asarray(res.results[c]["out"], np.float32) for c in range(N_CORES)], axis=0)
    return out
```

```python
import contextlib
import numpy as np
import concourse.bass as bass
import concourse.mybir as mybir
from concourse.bass_utils import run_bass_kernel_spmd

F32 = mybir.dt.float32
BF16 = mybir.dt.bfloat16
AF = mybir.ActivationFunctionType
ALU = mybir.AluOpType
AX = mybir.AxisListType

S_LEN = 2048
D = 1024
NT = 16
NS = 4
N_EXP = 32
ALPHA = 2.0 ** 0.25
LN_EPS = 1e-5
N_CORES = 8
N_INCH = 31
SLOT_B = 256
SLOT_SHIFT = 8
SUBS = SLOT_B // 128
N_OVF = (2 * S_LEN) // SLOT_B
N_SLOTS = N_EXP + N_OVF
I32 = mybir.dt.int32

COMPUTE = ("pe", "act", "dve", "pool")
ENGS = ("pe", "act", "dve", "pool", "sp")
PAGE = 2048


class View:
    __slots__ = ("ap", "arena", "ivals")

    def __init__(self, ap, arena, ivals):
        self.ap = ap
        self.arena = arena
        self.ivals = ivals


def _ivals(shape, esize, base, idx):
    nd = len(shape)
    rngs = []
    for d in range(nd):
        if d < len(idx):
            i = idx[d]
            if isinstance(i, int):
                rngs.append((i, i + 1))
            else:
                a = 0 if i.start is None else i.start
                b = shape[d] if i.stop is None else i.stop
                rngs.append((a, b))
        else:
            rngs.append((0, shape[d]))
    strides = [0] * nd
    s = esize
    for d in range(nd - 1, -1, -1):
        strides[d] = s
        s *= shape[d]
    tail = nd
    while tail > 0 and rngs[tail - 1] == (0, shape[tail - 1]):
        tail -= 1
    out = []
    if tail == 0:
        return [(base, base + s)]
    lead = rngs[: tail - 1]
    a, b = rngs[tail - 1]
    blk_lo = a * strides[tail - 1]
    blk_hi = b * strides[tail - 1]
    cnt = 1
    for (x, y) in lead:
        cnt *= (y - x)
    if cnt > 64:
        lo = base + sum(r[0] * strides[d] for d, r in enumerate(lead)) + blk_lo
        hi = base + sum((r[1] - 1) * strides[d] for d, r in enumerate(lead)) + blk_hi
        return [(lo, hi)]

    def rec(d, off):
        if d == tail - 1:
            out.append((base + off + blk_lo, base + off + blk_hi))
            return
        for i in range(lead[d][0], lead[d][1]):
            rec(d + 1, off + i * strides[d])
    rec(0, 0)
    return out


class Buf:
    def __init__(self, arena_name, ap_full, shape, esize, base_bytes):
        self.arena = arena_name
        self.ap_full = ap_full
        self.shape = tuple(shape)
        self.esize = esize
        self.base = base_bytes

    def v(self, *idx, p=None):
        full = (slice(None) if p is None else p,) + tuple(idx)
        ap = self.ap_full[full]
        if self.arena == "ps":
            return View(ap, self.arena, [(self.base, self.base + 2048)])
        return View(ap, self.arena, _ivals(self.shape, self.esize, self.base, idx))


class Sched:
    def __init__(self, nc, n_dma_sems=32):
        self.nc = nc
        self.streams = {e: [] for e in ENGS}
        self.cnt = {e: 0 for e in COMPUTE}
        self.pages = {}
        self.waited = {}
        self.n_dma_sems = n_dma_sems
        self.dma_issued = 0
        self.dma_slot_val = [0] * n_dma_sems

    def _touch(self, view, is_write, deps, own_sem=None):
        ps = view.arena == "ps"
        for (lo, hi) in view.ivals:
            for pg in range(lo // PAGE, (hi - 1) // PAGE + 1):
                d = self.pages.get((view.arena, pg))
                if not d:
                    continue
                for (rlo, rhi, sem, kind), val in d.items():
                    if rlo < hi and lo < rhi and (is_write or kind == "w" or (ps and sem != own_sem)):
                        deps.append((sem, val))

    def _record(self, view, is_write, token):
        sem, val = token
        kind = "w" if is_write else "r"
        for (lo, hi) in view.ivals:
            for pg in range(lo // PAGE, (hi - 1) // PAGE + 1):
                d = self.pages.setdefault((view.arena, pg), {})
                if is_write:
                    dead = [k for k in d if k[0] >= lo and k[1] <= hi]
                    for k in dead:
                        del d[k]
                d[(lo, hi, sem, kind)] = val

    def op(self, eng, fn, reads=(), writes=(), dma=False):
        deps = []
        own = None if dma else (eng,)
        for v in reads:
            self._touch(v, False, deps, own)
        for v in writes:
            self._touch(v, True, deps, own)
        if dma:
            slot = self.dma_issued % self.n_dma_sems
            self.dma_issued += 1
            prev = self.dma_slot_val[slot]
            if prev > 0:
                deps.append((("dma", slot), prev))
            self.dma_slot_val[slot] = prev + 16
            token = (("dma", slot), prev + 16)
            inc = 16
        else:
            self.cnt[eng] += 1
            token = ((eng,), self.cnt[eng])
            inc = 1
        need = {}
        for sem, val in deps:
            if (not dma) and eng == "pe" and sem == ("pe",):
                continue
            if need.get(sem, 0) < val:
                need[sem] = val
        waits = []
        for sem, val in need.items():
            if self.waited.get((eng, sem), 0) >= val:
                continue
            self.waited[(eng, sem)] = val
            waits.append((sem, val))
        self.streams[eng].append((waits, fn, token, inc))
        for v in reads:
            self._record(v, False, token)
        for v in writes:
            self._record(v, True, token)
        return token

    def emit(self, es, final_wait_eng="sp"):
        nc = self.nc
        sems = {}
        for e in COMPUTE:
            sems[(e,)] = es.enter_context(nc.semaphore("s_" + e))
        for i in range(self.n_dma_sems):
            sems[("dma", i)] = es.enter_context(nc.semaphore("s_dma%d" % i))
        block = es.enter_context(nc.Block())
        finals = []
        for e in COMPUTE:
            if self.cnt[e] > 0:
                finals.append(((e,), self.cnt[e]))
        for i in range(self.n_dma_sems):
            if self.dma_slot_val[i] > 0:
                finals.append((("dma", i), self.dma_slot_val[i]))

        def make(engname):
            stream = self.streams[engname]

            def body(eng):
                for waits, fn, token, inc in stream:
                    for sem, val in waits:
                        eng.wait_ge(sems[sem], val)
                    ins = fn(eng)
                    ins.then_inc(sems[token[0]], inc)
                if engname == final_wait_eng:
                    for sem, val in finals:
                        eng.wait_ge(sems[sem], val)
            return body

        block.tensor(make("pe"))
        block.scalar(make("act"))
        block.vector(make("dve"))
        block.gpsimd(make("pool"))
        block.sync(make("sp"))


ARENA_WORDS = 51200


def build_program(stop=99, dbg=False):
    nc = bass.Bass("TRN2", target_bir_lowering=False)
    dbg_list = []

    def din(name, shape, dt=F32):
        return nc.dram_tensor(name, list(shape), dt, kind="ExternalInput").ap()

    d_xT = din("xT", [D, S_LEN])
    d_x = din("x_tok", [S_LEN, D])
    d_win = din("w_in_r", [N_INCH, 128, 8, 128])
    d_bin = din("b_in_r", [128, N_INCH])
    d_bv = din("bv_bc", [128, 128])
    d_cos = din("cosT", [128, S_LEN])
    d_sin = din("sinT", [128, S_LEN])
    d_ec = din("ec", [128, 4, 16])
    d_wmix = din("wmix_r", [128, 4, 128])
    d_bmix = din("bmix_r", [128, 4])
    d_pscale = din("pscale_r", [128, 4])
    d_sink = din("sink_bc", [128, 8])
    d_wpb = din("wpb_r", [128, 4, D])
    d_wab = din("wab_r", [128, 4, D])
    d_wo = din("wo_r", [128, 8, D])
    d_ln1g = din("ln1g_bc", [128, D])
    d_ln1b = din("ln1b_bc", [128, D])
    d_ln2g = din("ln2g_bc", [128, D])
    d_ln2b = din("ln2b_bc", [128, D])
    d_wr = din("wr_r", [128, 8, 36])
    d_br = din("br_bc", [128, 36])
    if stop >= 5:
        d_wg = din("wg_r", [N_EXP, 128, 8, 512])
        d_wu = din("wu_r", [N_EXP, 128, 8, 512])
        d_wd = din("wd_r", [N_EXP, 128, 4, D])
        d_wg_rows = d_wg.rearrange("e p k m -> (e p) (k m)")
        d_wu_rows = d_wu.rearrange("e p k m -> (e p) (k m)")
        d_wd_rows = d_wd.rearrange("e p k m -> (e p) (k m)")
    d_xin = nc.dram_tensor("xin_scratch", [N_SLOTS * SLOT_B, D], BF16, kind="Internal").ap()
    d_yb = nc.dram_tensor("yb_scratch", [N_SLOTS * SLOT_B, D], F32, kind="Internal").ap()
    d_out = nc.dram_tensor("out", [S_LEN, D], F32, kind="ExternalOutput").ap()
    d_x2s = nc.dram_tensor("x2_scratch", [S_LEN, D], F32, kind=("ExternalOutput" if dbg else "Internal")).ap()

    es = contextlib.ExitStack()
    with es:
        arena = es.enter_context(nc.sbuf_tensor("arena", [128, ARENA_WORDS], F32))
        banks_t = [es.enter_context(nc.psum_tensor("ps%d" % i, [128, 512], F32)) for i in range(8)]
        S = Sched(nc)

        def buf(off, shape, dt=F32):
            n = int(np.prod(shape))
            if dt == F32:
                words = n
                ap = arena[:, off:off + words]
                esize = 4
            elif dt == I32:
                words = n
                ap = arena[:, off:off + words].bitcast(I32)
                esize = 4
            else:
                assert n % 2 == 0
                words = n // 2
                ap = arena[:, off:off + words].bitcast(BF16)
                esize = 2
            if len(shape) > 1:
                names = " ".join("d%d" % i for i in range(len(shape)))
                kw = {"d%d" % i: shape[i] for i in range(1, len(shape))}
                ap = ap.rearrange("p (%s) -> p %s" % (names, names), **kw)
            return Buf("sb", ap, shape, esize, off * 4), off + words

        def psbuf(i, shape, dt=F32):
            n_ = int(np.prod(shape))
            if dt == F32:
                ap = banks_t[i][:, 0:n_]
                esize = 4
            else:
                ap = banks_t[i][:].bitcast(BF16)[:, 0:n_]
                esize = 2
            if len(shape) > 1:
                names = " ".join("d%d" % j for j in range(len(shape)))
                kw = {"d%d" % j: shape[j] for j in range(1, len(shape))}
                ap = ap.rearrange("p (%s) -> p %s" % (names, names), **kw)
            return Buf("ps", ap, shape, esize, i * 2048)

        PSF = [psbuf(i, [512]) for i in range(8)]
        PSB = [psbuf(i, [1024], BF16) for i in range(8)]
        ps_rr = [0]

        def next_bank():
            i = ps_rr[0] % 8
            ps_rr[0] += 1
            return i

        def dview(ap, name, lo, hi):
            return View(ap, name, [(lo, hi)])

        def mm(out, lhsT, rhs, start, stop):
            S.op("pe", lambda e: e.matmul(out.ap, lhsT.ap, rhs.ap, start=start, stop=stop),
                 reads=[lhsT, rhs], writes=[out])

        def tr(out, in_, ident):
            S.op("pe", lambda e: e.transpose(out.ap, in_.ap, ident.ap), reads=[in_, ident], writes=[out])

        def act(out, in_, func, bias=0.0, scale=1.0, accum=None, eng="act"):
            rd = [in_]
            b = bias
            s = scale
            if isinstance(bias, View):
                rd.append(bias)
                b = bias.ap
            if isinstance(scale, View):
                rd.append(scale)
                s = scale.ap
            wr = [out]
            if accum is not None:
                wr.append(accum)
                S.op(eng, lambda e: e.activation(out=out.ap, in_=in_.ap, func=func, bias=b, scale=s,
                                                 accum_out=accum.ap), reads=rd, writes=wr)
            else:
                S.op(eng, lambda e: e.activation(out=out.ap, in_=in_.ap, func=func, bias=b, scale=s),
                     reads=rd, writes=wr)

        def tt(eng, out, in0, in1, op):
            S.op(eng, lambda e: e.tensor_tensor(out=out.ap, in0=in0.ap, in1=in1.ap, op=op),
                 reads=[in0, in1], writes=[out])

        def ts(eng, out, in0, s1, op0, s2=None, op1=None):
            rd = [in0]
            a1 = s1
            a2 = s2
            if isinstance(s1, View):
                rd.append(s1)
                a1 = s1.ap
            if isinstance(s2, View):
                rd.append(s2)
                a2 = s2.ap
            if op1 is None:
                S.op(eng, lambda e: e.tensor_scalar(out=out.ap, in0=in0.ap, scalar1=a1, scalar2=None, op0=op0),
                     reads=rd, writes=[out])
            else:
                S.op(eng, lambda e: e.tensor_scalar(out=out.ap, in0=in0.ap, scalar1=a1, scalar2=a2, op0=op0, op1=op1),
                     reads=rd, writes=[out])

        def stt(out, in0, scalar, in1, op0, op1):
            rd = [in0, in1]
            sc = scalar
            if isinstance(scalar, View):
                rd.append(scalar)
                sc = scalar.ap
            S.op("dve", lambda e: e.scalar_tensor_tensor(out=out.ap, in0=in0.ap, scalar=sc, in1=in1.ap, op0=op0, op1=op1),
                 reads=rd, writes=[out])

        def cp(eng, out, in_):
            if eng == "act":
                S.op(eng, lambda e: e.copy(out=out.ap, in_=in_.ap), reads=[in_], writes=[out])
            else:
                S.op(eng, lambda e: e.tensor_copy(out=out.ap, in_=in_.ap), reads=[in_], writes=[out])

        def memset(eng, out, val):
            S.op(eng, lambda e: e.memset(out.ap, val), writes=[out])

        def dma(eng, out, in_):
            S.op(eng, lambda e: e.dma_start(out=out.ap, in_=in_.ap), reads=[in_], writes=[out], dma=True)

        A0, B0, C0, D0, E0, F0 = 0, 16384, 28672, 36864, 45056, 50176
        o = F0
        ident_f, o = buf(o, [128])
        ident_b, o = buf(o, [128], BF16)
        wmix, o = buf(o, [4, 128], BF16)
        wr_f, o = buf(o, [8, 36])
        b_in, o = buf(o, [N_INCH])
        bmix, o = buf(o, [4])
        pscale, o = buf(o, [4])
        bms, o = buf(o, [4])
        esink, o = buf(o, [8])
        br_bc, o = buf(o, [36])
        ec, o = buf(o, [4, 16])
        den, o = buf(o, [4])
        rden, o = buf(o, [4])
        assert o <= ARENA_WORDS, o
        xT, _ = buf(D0, [8, S_LEN], BF16)
        mixT, _ = buf(C0, [4, S_LEN], BF16)
        oT, _ = buf(C0 + 4096, [4, S_LEN], BF16)
        hT = [buf(C0 + 1024 * i, [4, 512], BF16)[0] for i in range(8)]
        o = B0
        wch = []
        for i in range(4):
            b_, o = buf(o, [8, 128], BF16)
            wch.append(b_)
        wpb, o = buf(o, [4, D], BF16)
        wab, o = buf(o, [4, D], BF16)
        wo, o = buf(o, [8, D], BF16)
        mT, o = buf(o, [8, 512], BF16)
        assert o == C0
        wexp = []
        o = B0
        for i in range(2):
            g_, o = buf(o, [8, 512], BF16)
            u_, o = buf(o, [8, 512], BF16)
            d_, o = buf(o, [4, D], BF16)
            wexp.append((g_, u_, d_))
        o = A0
        PADW = S_LEN + 16
        uT, o = buf(o, [PADW])
        sA, o = buf(o, [PADW])
        sB, o = buf(o, [PADW])
        cosT, o = buf(o, [S_LEN])
        sinT, o = buf(o, [S_LEN])
        qT, o = buf(o, [4, S_LEN], BF16)
        kT, o = buf(o, [S_LEN], BF16)
        otok = []
        for i in range(2):
            b_, o = buf(o, [512], BF16)
            otok.append(b_)
        assert o <= B0, o
        o = A0
        sg0, o = buf(o, [512])
        sg1, o = buf(o, [512])
        t0b, o = buf(o, [512])
        t1b, o = buf(o, [512])
        xtok = []
        abuf = []
        for i in range(4):
            b_, o = buf(o, [D])
            xtok.append(b_)
        for i in range(2):
            b_, o = buf(o, [D])
            abuf.append(b_)
        x2Tf, o = buf(o, [8, 128])
        ln1g, o = buf(o, [D])
        ln1b, o = buf(o, [D])
        stats, o = buf(o, [2, 6])
        mv, o = buf(o, [2])
        rstd, o = buf(o, [1])
        lg, o = buf(o, [36])
        rt, o = buf(o, [64])
        assert o <= B0, o
        o = E0 + 1536
        OH1f, o = buf(o, [NT, 32])
        OH2f, o = buf(o, [NT, 32])
        w12, o = buf(o, [2, NT])
        assert o <= F0, o
        o = A0
        prefix, o = buf(o, [NT, 32])
        slotpos, o = buf(o, [NT, 32])
        tmp512, o = buf(o, [NT, 32])
        ind, o = buf(o, [NT, 32], BF16)
        ones_bf, o = buf(o, [128], BF16)
        stri_bf, o = buf(o, [128], BF16)
        cnt, o = buf(o, [32])
        cnt_i, o = buf(o, [32], I32)
        nb_i, o = buf(o, [32], I32)
        nb_f, o = buf(o, [32])
        pendb, o = buf(o, [32])
        pstart, o = buf(o, [32])
        zeros32, o = buf(o, [32])
        junk32, o = buf(o, [32])
        es_f, o = buf(o, [N_SLOTS])
        widx_f, o = buf(o, [N_SLOTS])
        widx_i, o = buf(o, [N_SLOTS], I32)
        d1f, o = buf(o, [NT])
        d2f, o = buf(o, [NT])
        d1i, o = buf(o, [NT], I32)
        d2i, o = buf(o, [NT], I32)
        cm_f, o = buf(o, [32])
        basep, o = buf(o, [32])
        basep_i, o = buf(o, [32], I32)
        pidx, o = buf(o, [1])
        pidx_i, o = buf(o, [1], I32)
        o += (-o) % 2
        o_rg = o
        rg1, rg2 = [], []
        for i in range(2):
            b_, o_rg = buf(o_rg, [D])
            rg1.append(b_)
            b_, o_rg = buf(o_rg, [D])
            rg2.append(b_)
        xr = []
        for i in range(2):
            b_, o = buf(o, [D])
            xr.append(b_)
        xb, xbT = [], []
        for i in range(4):
            b_, o = buf(o, [D], BF16)
            xb.append(b_)
        for i in range(4):
            b_, o = buf(o, [8, 128], BF16)
            xbT.append(b_)
        assert o <= A0 + 8704, o
        o = C0
        hTs, hTs4, ybs = [], [], []
        for i in range(4):
            h4, _ = buf(o, [4, 128], BF16)
            b_, o = buf(o, [512], BF16)
            hTs.append(b_)
            hTs4.append(h4)
        for i in range(4):
            b_, o = buf(o, [D])
            ybs.append(b_)
        assert o <= D0, o
        wexp2 = []
        o = B0
        for i in range(2):
            g_, o = buf(o, [4096], BF16)
            u_, o = buf(o, [4096], BF16)
            d_, o = buf(o, [4096], BF16)
            wexp2.append((g_, u_, d_))
        o = A0 + 8704
        g_, o = buf(o, [8, 512], BF16)
        u_, o = buf(o, [8, 512], BF16)
        d_, o = buf(o, [4, D], BF16)
        wexp.append((g_, u_, d_))
        assert o <= B0, o
        o = A0 + 8704
        g_, o = buf(o, [4096], BF16)
        u_, o = buf(o, [4096], BF16)
        d_, o = buf(o, [4096], BF16)
        wexp2.append((g_, u_, d_))
        o = D0
        g_, o = buf(o, [8, 512], BF16)
        u_, o = buf(o, [8, 512], BF16)
        d_, o = buf(o, [4, D], BF16)
        wexp.append((g_, u_, d_))
        assert o <= E0, o
        o = D0
        g_, o = buf(o, [4096], BF16)
        u_, o = buf(o, [4096], BF16)
        d_, o = buf(o, [4096], BF16)
        wexp2.append((g_, u_, d_))
        NWS = 4
        wexp = [wexp[0], wexp[1], wexp[3], wexp[2]]
        wexp2 = [wexp2[0], wexp2[1], wexp2[3], wexp2[2]]
        xr.append(buf(C0 + 1024, [D])[0])
        xr.append(buf(C0 + 2048, [D])[0])
        o = E0
        vaug, o = buf(o, [NT, 2, 65], BF16)
        pT = []
        for i in range(2):
            b_, o = buf(o, [3, 512], BF16)
            pT.append(b_)
        pooled, o = buf(o, [S_LEN], BF16)
        rtmp1, o = buf(o, [512])
        rtmp2, o = buf(o, [512])
        bv_bc, o = buf(o, [128])
        assert o <= F0, o
        o = E0
        sl = []
        for i in range(2):
            b_, o = buf(o, [512])
            sl.append(b_)
        coef, o = buf(o, [NT, 32])
        assert o <= F0, o
        o = B0
        ln2g, o = buf(o, [D])
        ln2b, o = buf(o, [D])
        x2rb = []
        a2 = []
        for i in range(4):
            b_, o = buf(o, [D])
            x2rb.append(b_)
        for i in range(2):
            b_, o = buf(o, [D])
            a2.append(b_)
        stats2, o = buf(o, [2, 6])
        mv2, o = buf(o, [2])
        rstd2, o = buf(o, [1])
        nbias2, o = buf(o, [1])
        assert o <= C0

        if dbg:
            def dout(name, shape, dt):
                return nc.dram_tensor(name, list(shape), dt, kind="ExternalOutput").ap()
            if 1 <= stop <= 4:
                dbg_list.append((dout("dbg_mixT", [128, 4, S_LEN], BF16), mixT.v()))
            if 2 <= stop <= 3:
                dbg_list.append((dout("dbg_qT", [128, 4, S_LEN], BF16), qT.v()))
                dbg_list.append((dout("dbg_kT", [128, S_LEN], BF16), kT.v()))
                dbg_list.append((dout("dbg_vaug", [128, NT, 2, 65], BF16), vaug.v()))
            if 3 <= stop <= 4:
                dbg_list.append((dout("dbg_oT", [128, 4, S_LEN], BF16), oT.v()))
            if stop >= 4:
                dbg_list.append((dout("dbg_coef", [128, NT, 32], F32), coef.v()))
            if stop == 4:
                dbg_list.append((dout("dbg_x2T", [128, 8, S_LEN], BF16), xT.v()))
            if stop >= 4.5:
                dbg_list.append((dout("dbg_d1", [128, NT], F32), d1f.v()))
                dbg_list.append((dout("dbg_d2", [128, NT], F32), d2f.v()))
                dbg_list.append((dout("dbg_es", [128, N_SLOTS], F32), es_f.v()))
                dbg_list.append((dout("dbg_w12", [128, 2, NT], F32), w12.v()))

        def D_(ap, name="dram_in", lo=0, hi=1):
            return View(ap, name, [(lo, hi)])

        def body():
            dma("sp", b_in.v(), D_(d_bin))
            dma("sp", bmix.v(), D_(d_bmix))
            dma("sp", pscale.v(), D_(d_pscale))
            dma("sp", esink.v(), D_(d_sink))
            dma("sp", br_bc.v(), D_(d_br))
            dma("sp", ec.v(), D_(d_ec))
            dma("sp", wr_f.v(), D_(d_wr))
            dma("sp", bv_bc.v(), D_(d_bv))
            dma("sp", cosT.v(), D_(d_cos))
            dma("sp", sinT.v(), D_(d_sin))
            dma("pool", wmix.v(), D_(d_wmix))
            for k in range(8):
                dma("pool", xT.v(k), D_(d_xT[k * 128:(k + 1) * 128, :]))
            memset("pool", ident_f.v(), 0.0)
            S.op("pool", lambda e: e.affine_select(out=ident_f.v().ap, in_=ident_f.v().ap, pattern=[[-1, 128]],
                                                   compare_op=ALU.not_equal, fill=1.0, base=0, channel_multiplier=1),
                 reads=[ident_f.v()], writes=[ident_f.v()])
            cp("dve", ident_b.v(), ident_f.v())
            tt("dve", bms.v(), bmix.v(), pscale.v(), ALU.mult)
            act(esink.v(), esink.v(), AF.Exp)
            memset("pool", vaug.v(), 1.0)
            memset("dve", uT.v(), 0.0)

            wch_seq = ([0, 1, 2, 3, 4, 8, 5, 9, 6, 10, 7, 11, 12, 13, 14]
                       + [c for _n in range(NS) for m_ in range(8) for c in (15 + m_, 23 + m_)])
            wch_pos = [0]
            wch_issued = [0]
            WCH_AHEAD = 2

            def load_wchunk(ch):
                pos = wch_pos[0]
                assert wch_seq[pos] == ch, (pos, ch)
                while wch_issued[0] < min(len(wch_seq), pos + 1 + WCH_AHEAD):
                    j = wch_issued[0]
                    dma("pool", wch[j % 4].v(), D_(d_win[wch_seq[j]]))
                    wch_issued[0] += 1
                wch_pos[0] += 1
                return wch[pos % 4]

            def inproj_fm(ch):
                w = load_wchunk(ch)
                bks = [next_bank() for _ in range(NS)]
                for k in range(8):
                    for n in range(NS):
                        mm(PSF[bks[n]].v(), w.v(k), xT.v(k, slice(n * 512, (n + 1) * 512)), k == 0, k == 7)
                return bks

            for g in range(4):
                wdw = 2 ** (g + 1)
                bks = inproj_fm(g)
                for n in range(NS):
                    act(uT.v(slice(8 + n * 512, 8 + (n + 1) * 512)), PSF[bks[n]].v(), AF.Identity,
                        bias=b_in.v(slice(g, g + 1)))
                W_ = PADW
                src = uT
                lvl = [(1, sA), (2, sB), (4, sA), (8, sB)]
                tt("dve", sA.v(slice(1, W_)), uT.v(slice(0, W_ - 1)), uT.v(slice(1, W_)), ALU.add)
                cur = sA
                lo, hi = 1, W_
                half = 1
                for li in range(1, g + 1):
                    dst = sB if cur is sA else sA
                    nlo, nhi = lo + half, hi - half
                    tt("dve", dst.v(slice(nlo, nhi)), cur.v(slice(nlo - half, nhi - half)),
                       cur.v(slice(nlo + half, nhi + half)), ALU.add)
                    cur = dst
                    lo, hi = nlo, nhi
                    half *= 2
                assert lo <= 8 and hi >= S_LEN + 8
                ts("dve", cur.v(slice(8, 8 + S_LEN)), cur.v(slice(8, 8 + S_LEN)), 1.0 / wdw, ALU.mult)
                tt("dve", cur.v(slice(8, 16)), cur.v(slice(8, 16)), ec.v(g, slice(0, 8)), ALU.mult)
                tt("dve", cur.v(slice(S_LEN, S_LEN + 8)), cur.v(slice(S_LEN, S_LEN + 8)), ec.v(g, slice(8, 16)), ALU.mult)
                tt("dve", pooled.v(), cur.v(slice(8, 8 + S_LEN)), uT.v(slice(8, 8 + S_LEN)), ALU.subtract)
                for n in range(NS):
                    b = next_bank()
                    mm(PSF[b].v(), wmix.v(g), pooled.v(slice(n * 512, (n + 1) * 512)), True, True)
                    act(mixT.v(g, slice(n * 512, (n + 1) * 512)), PSF[b].v(), AF.Identity,
                        bias=bms.v(slice(g, g + 1)), scale=pscale.v(slice(g, g + 1)))

            if stop <= 1:
                return
            def rope_chunk(ch_main, ch_swap, dst_fn):
                bk_m = inproj_fm(ch_main)
                bk_s = inproj_fm(ch_swap)
                for n in range(NS):
                    cs = slice(n * 512, (n + 1) * 512)
                    stt(rtmp1.v(), PSF[bk_m[n]].v(), b_in.v(slice(ch_main, ch_main + 1)), cosT.v(cs), ALU.add, ALU.mult)
                    stt(rtmp2.v(), PSF[bk_s[n]].v(), b_in.v(slice(ch_swap, ch_swap + 1)), sinT.v(cs), ALU.add, ALU.mult)
                    tt("pool", dst_fn(cs), rtmp1.v(), rtmp2.v(), ALU.add)

            for c in range(4):
                rope_chunk(4 + c, 8 + c, lambda cs, c=c: qT.v(c, cs))
            rope_chunk(12, 13, lambda cs: kT.v(cs))
            wv = load_wchunk(14)
            for t in range(NT):
                b = next_bank()
                pv = PSF[b]
                for k in range(8):
                    mm(pv.v(slice(0, 128)), xT.v(k, slice(t * 128, (t + 1) * 128)), wv.v(k), k == 0, k == 7)
                for h in range(2):
                    tt("dve", vaug.v(t, h, slice(0, 64)), pv.v(slice(h * 64, (h + 1) * 64)),
                       bv_bc.v(slice(h * 64, (h + 1) * 64)), ALU.add)

            if stop <= 2:
                return
            for i in range(NT):
                ot = otok[i % 2]
                qs = slice(i * 128, (i + 1) * 128)
                for g in range(2):
                    pg = slice(64 * g, 64 * g + 64)
                    pt = pT[(2 * i + g) % 2]
                    js = [j for j in (i - 1, i, i + 1) if 0 <= j < NT]
                    for jj, j in enumerate(js):
                        b = next_bank()
                        for hh in range(4):
                            mm(PSF[b].v(slice(hh * 128, (hh + 1) * 128)),
                               kT.v(slice(j * 128, (j + 1) * 128), p=pg), qT.v(hh, qs, p=pg), True, True)
                        act(pt.v(jj), PSF[b].v(), AF.Exp, scale=0.125)
                        if j == i - 1:
                            S.op("pool", lambda e, v=pt.v(jj): e.affine_select(
                                out=v.ap, in_=v.ap, pattern=[[0, 4], [-1, 128]], compare_op=ALU.is_ge,
                                fill=0.0, base=0, channel_multiplier=1), reads=[pt.v(jj)], writes=[pt.v(jj)])
                        elif j == i + 1:
                            S.op("pool", lambda e, v=pt.v(jj): e.affine_select(
                                out=v.ap, in_=v.ap, pattern=[[0, 4], [1, 128]], compare_op=ALU.is_ge,
                                fill=0.0, base=0, channel_multiplier=-1), reads=[pt.v(jj)], writes=[pt.v(jj)])
                    bo = next_bank()
                    po = psbuf(bo, [4, 65])
                    for hh in range(4):
                        for jj, j in enumerate(js):
                            mm(po.v(hh), pt.v(jj, slice(hh * 128, (hh + 1) * 128)), vaug.v(j, g),
                               jj == 0, jj == len(js) - 1)
                    tt("dve", den.v(), po.v(slice(0, 4), 64), esink.v(slice(4 * g, 4 * g + 4)), ALU.add)
                    S.op("dve", lambda e: e.reciprocal(out=rden.v().ap, in_=den.v().ap), reads=[den.v()], writes=[rden.v()])
                    for hh in range(4):
                        hd = 4 * g + hh
                        ts("dve", ot.v(slice(hd * 64, (hd + 1) * 64)), po.v(hh, slice(0, 64)),
                           rden.v(slice(hh, hh + 1)), ALU.mult)
                bt = next_bank()
                ptb = psbuf(bt, [4, 256], BF16)
                for c in range(4):
                    tr(ptb.v(c, slice(0, 128)), ot.v(slice(c * 128, (c + 1) * 128)), ident_b.v())
                cp("act", oT.v(slice(0, 4), qs), ptb.v(slice(0, 4), slice(0, 128)))

            if stop <= 3:
                return
            dma("pool", wpb.v(), D_(d_wpb))
            dma("pool", wab.v(), D_(d_wab))
            dma("pool", wo.v(), D_(d_wo))
            dma("sp", ln1g.v(), D_(d_ln1g))
            dma("sp", ln1b.v(), D_(d_ln1b))

            def layer_norm(a, stats_b, mv_b, rstd_b, g_b, b_b, out, mul_eng="pool", nbias_b=None):
                for c in range(2):
                    S.op("dve", lambda e, c=c: e.bn_stats(out=stats_b.v(c).ap, in_=a.v(slice(c * 512, (c + 1) * 512)).ap),
                         reads=[a.v(slice(c * 512, (c + 1) * 512))], writes=[stats_b.v(c)])
                S.op("dve", lambda e: e.bn_aggr(out=mv_b.v().ap, in_=stats_b.v().ap), reads=[stats_b.v()], writes=[mv_b.v()])
                act(rstd_b.v(), mv_b.v(slice(1, 2)), AF.Sqrt, bias=LN_EPS)
                S.op("dve", lambda e: e.reciprocal(out=rstd_b.v().ap, in_=rstd_b.v().ap), reads=[rstd_b.v()], writes=[rstd_b.v()])
                if nbias_b is None:
                    ts("dve", a.v(), a.v(), mv_b.v(slice(0, 1)), ALU.subtract, rstd_b.v(), ALU.mult)
                else:
                    ts("dve", nbias_b.v(), mv_b.v(slice(0, 1)), rstd_b.v(), ALU.mult, -1.0, ALU.mult)
                    act(a.v(), a.v(), AF.Identity, bias=nbias_b.v(), scale=rstd_b.v())
                tt(mul_eng, a.v(), a.v(), g_b.v(), ALU.mult)
                tt("dve", out.v(), a.v(), b_b.v(), ALU.add)

            def stage_A(n):
                cs = slice(n * 512, (n + 1) * 512)
                for m in range(8):
                    ms = slice(m * 128, (m + 1) * 128)
                    w0 = load_wchunk(15 + m)
                    w1 = load_wchunk(23 + m)
                    byp, bya, bg0, bg1 = next_bank(), next_bank(), next_bank(), next_bank()
                    for k in range(4):
                        mm(PSF[byp].v(), wpb.v(k, ms), mixT.v(k, cs), k == 0, k == 3)
                    for k in range(4):
                        mm(PSF[bya].v(), wab.v(k, ms), oT.v(k, cs), k == 0, k == 3)
                    for k in range(8):
                        mm(PSF[bg0].v(), w0.v(k), xT.v(k, cs), k == 0, k == 7)
                    for k in range(8):
                        mm(PSF[bg1].v(), w1.v(k), xT.v(k, cs), k == 0, k == 7)
                    act(sg0.v(), PSF[bg0].v(), AF.Sigmoid, bias=b_in.v(slice(15 + m, 16 + m)))
                    act(sg1.v(), PSF[bg1].v(), AF.Sigmoid, bias=b_in.v(slice(23 + m, 24 + m)))
                    tt("dve", t0b.v(), PSF[byp].v(), sg0.v(), ALU.mult)
                    tt("dve", t1b.v(), PSF[bya].v(), sg1.v(), ALU.mult)
                    tt("pool", mT.v(m), t0b.v(), t1b.v(), ALU.add)

            def stage_B(n):
                for tl in range(4):
                    t = n * 4 + tl
                    dma("sp", xtok[t % 4].v(), D_(d_x[t * 128:(t + 1) * 128, :]))
                for tl in range(4):
                    t = n * 4 + tl
                    xt = xtok[t % 4]
                    ab = abuf[t % 2]
                    for hf in range(2):
                        b = next_bank()
                        for m in range(8):
                            mm(PSF[b].v(), mT.v(m, slice(tl * 128, (tl + 1) * 128)), wo.v(m, slice(hf * 512, (hf + 1) * 512)),
                               m == 0, m == 7)
                        stt(ab.v(slice(hf * 512, (hf + 1) * 512)), xt.v(slice(hf * 512, (hf + 1) * 512)), ALPHA,
                            PSF[b].v(), ALU.mult, ALU.add)
                    layer_norm(ab, stats, mv, rstd, ln1g, ln1b, xt)
                    dma("sp", View(d_x2s[t * 128:(t + 1) * 128, :], "x2s", [(t, t + 1)]), xt.v())

            def stage_C(n):
                for tl in range(4):
                    t = n * 4 + tl
                    xt = xtok[t % 4]
                    b0, b1 = next_bank(), next_bank()
                    for k in range(8):
                        bb = b0 if k < 4 else b1
                        kk = k % 4
                        tr(PSF[bb].v(slice(kk * 128, (kk + 1) * 128)), xt.v(slice(k * 128, (k + 1) * 128)), ident_f.v())
                    for hb, bb in enumerate((b0, b1)):
                        pview = psbuf(bb, [4, 128])
                        cp("act", x2Tf.v(slice(hb * 4, hb * 4 + 4)), pview.v())
                    br_ = next_bank()
                    for k in range(8):
                        mm(PSF[br_].v(slice(0, 36)), x2Tf.v(k), wr_f.v(k), k == 0, k == 7)
                    tt("dve", lg.v(), PSF[br_].v(slice(0, 36)), br_bc.v(), ALU.add)
                    R = lambda a, b=None: rt.v(slice(a, (a + 1) if b is None else b))
                    S.op("dve", lambda e: e.tensor_reduce(out=R(0).ap, in_=lg.v(slice(0, 4)).ap, axis=AX.X, op=ALU.max, negate=True),
                         reads=[lg.v(slice(0, 4))], writes=[R(0)])
                    act(R(7, 11), lg.v(slice(0, 4)), AF.Exp, bias=R(0), accum=R(1))
                    S.op("dve", lambda e: e.reciprocal(out=R(2).ap, in_=R(1).ap), reads=[R(1)], writes=[R(2)])
                    ts("dve", R(3, 7), lg.v(slice(0, 4)), R(0), ALU.add, 0.0, ALU.is_ge)
                    ts("dve", R(11, 19), lg.v(slice(4, 12)), R(3), ALU.mult)
                    for gg in range(1, 4):
                        stt(R(11, 19), lg.v(slice(4 + 8 * gg, 12 + 8 * gg)), R(3 + gg), R(11, 19), ALU.mult, ALU.add)
                    S.op("dve", lambda e: e.tensor_reduce(out=R(19).ap, in_=R(11, 19).ap, axis=AX.X, op=ALU.max),
                         reads=[R(11, 19)], writes=[R(19)])
                    ts("dve", R(20, 28), R(11, 19), R(19), ALU.is_ge)
                    stt(R(28, 36), R(20, 28), -1e30, R(11, 19), ALU.mult, ALU.add)
                    S.op("dve", lambda e: e.tensor_reduce(out=R(36).ap, in_=R(28, 36).ap, axis=AX.X, op=ALU.max),
                         reads=[R(28, 36)], writes=[R(36)])
                    ts("dve", R(37, 45), R(28, 36), R(36), ALU.is_ge)
                    tt("dve", R(45), R(36), R(19), ALU.subtract)
                    act(R(46), R(45), AF.Exp)
                    ts("dve", R(47), R(46), 1.0, ALU.add)
                    S.op("dve", lambda e: e.reciprocal(out=R(47).ap, in_=R(47).ap), reads=[R(47)], writes=[R(47)])
                    tt("dve", R(48), R(47), R(2), ALU.mult)
                    tt("dve", R(49), R(48), R(46), ALU.mult)
                    ts("dve", R(50, 58), R(20, 28), R(48), ALU.mult)
                    stt(R(50, 58), R(37, 45), R(49), R(50, 58), ALU.mult, ALU.add)
                    for gg in range(4):
                        ts("dve", coef.v(t, slice(8 * gg, 8 * gg + 8)), R(50, 58), R(3 + gg), ALU.mult)
                        ts("dve", OH1f.v(t, slice(8 * gg, 8 * gg + 8)), R(20, 28), R(3 + gg), ALU.mult)
                        ts("dve", OH2f.v(t, slice(8 * gg, 8 * gg + 8)), R(37, 45), R(3 + gg), ALU.mult)
                    cp("dve", w12.v(0, slice(t, t + 1)), R(48))
                    cp("dve", w12.v(1, slice(t, t + 1)), R(49))

            stage_A(0)
            for n in range(NS):
                stage_B(n)
                if n + 1 < NS:
                    stage_A(n + 1)
                stage_C(n)

            if stop <= 4.5:
                return
            def load_w_static(s_):
                wg_b, wu_b, wd_b = wexp[s_ % NWS]
                dma("pool", wg_b.v(), D_(d_wg[s_]))
                dma("pool", wu_b.v(), D_(d_wu[s_]))
                dma("pool", wd_b.v(), D_(d_wd[s_]))
            if stop >= 5:
                for s_ in range(NWS):
                    load_w_static(s_)
            tt("dve", ind.v(), OH1f.v(), OH2f.v(), ALU.add)
            memset("pool", ones_bf.v(), 1.0)
            memset("pool", stri_bf.v(), 1.0)
            S.op("pool", lambda e: e.affine_select(out=stri_bf.v().ap, in_=stri_bf.v().ap, pattern=[[1, 128]],
                                                   compare_op=ALU.is_ge, fill=0.0, base=-1, channel_multiplier=-1),
                 reads=[stri_bf.v()], writes=[stri_bf.v()])
            memset("dve", zeros32.v(), 0.0)
            S.op("pool", lambda e: e.iota(pidx_i.v().ap, [[0, 1]], base=0, channel_multiplier=1), writes=[pidx_i.v()])
            cp("dve", pidx.v(), pidx_i.v())
            pb, cb = next_bank(), next_bank()
            for i in range(NT):
                for j in range(i + 1):
                    mm(PSF[pb].v(slice(i * 32, (i + 1) * 32)), (ones_bf if j < i else stri_bf).v(), ind.v(j), j == 0, j == i)
            for j in range(NT):
                mm(PSF[cb].v(slice(0, 32)), ones_bf.v(), ind.v(j), j == 0, j == NT - 1)
            cp("dve", prefix.v(), psbuf(pb, [NT, 32]).v())
            cp("dve", cnt.v(), PSF[cb].v(slice(0, 32)))
            ts("dve", cm_f.v(), cnt.v(), -float(SLOT_B), ALU.add, 0.0, ALU.max)
            cp("dve", cnt_i.v(), cm_f.v())
            ts("dve", nb_i.v(), cnt_i.v(), SLOT_B - 1, ALU.add)
            ts("dve", nb_i.v(), nb_i.v(), SLOT_SHIFT, ALU.arith_shift_right)
            cp("dve", nb_f.v(), nb_i.v())
            S.op("dve", lambda e: e.tensor_tensor_scan(out=pendb.v().ap, data0=zeros32.v().ap, data1=nb_f.v().ap,
                                                       initial=0.0, op0=ALU.add, op1=ALU.add),
                 reads=[zeros32.v(), nb_f.v()], writes=[pendb.v()])
            S.op("pool", lambda e: e.iota(basep_i.v().ap, [[SLOT_B, N_EXP]], base=0, channel_multiplier=0),
                 writes=[basep_i.v()])
            cp("dve", basep.v(), basep_i.v())
            tt("dve", pstart.v(), pendb.v(), nb_f.v(), ALU.subtract)
            ts("dve", pstart.v(), pstart.v(), float(SLOT_B), ALU.mult, float(N_EXP * SLOT_B - SLOT_B), ALU.add)
            tt("dve", pstart.v(), pstart.v(), basep.v(), ALU.subtract)
            for s_ in range(N_OVF):
                S.op("dve", lambda e, s_=s_: e.tensor_scalar(out=junk32.v().ap, in0=pendb.v().ap, scalar1=float(s_), scalar2=0.0,
                                                             op0=ALU.is_le, op1=ALU.add, accum_out=es_f.v(slice(s_, s_ + 1)).ap),
                     reads=[pendb.v()], writes=[junk32.v(), es_f.v(slice(s_, s_ + 1))])
            ts("dve", widx_f.v(slice(0, N_OVF)), es_f.v(slice(0, N_OVF)), 128.0, ALU.mult, pidx.v(), ALU.add)
            cp("dve", widx_i.v(slice(0, N_OVF)), widx_f.v(slice(0, N_OVF)))
            for t in range(NT):
                ts("dve", slotpos.v(t), prefix.v(t), float(SLOT_B), ALU.is_ge)
                tt("dve", slotpos.v(t), slotpos.v(t), pstart.v(), ALU.mult)
                tt("dve", slotpos.v(t), slotpos.v(t), prefix.v(t), ALU.add)
                tt("dve", slotpos.v(t), slotpos.v(t), basep.v(), ALU.add)
            for (ohf, dfl, din_) in ((OH1f, d1f, d1i), (OH2f, d2f, d2i)):
                tt("dve", tmp512.v(), ohf.v(), slotpos.v(), ALU.mult)
                S.op("dve", lambda e, dfl=dfl: e.tensor_reduce(out=dfl.v().ap, in_=tmp512.v().ap, axis=AX.X, op=ALU.add),
                     reads=[tmp512.v()], writes=[dfl.v()])
                cp("dve", din_.v(), dfl.v())
            if stop <= 4.8:
                return
            for t in range(NT):
                xr_ = xr[t % 4]
                dma("sp", xr_.v(), View(d_x2s[t * 128:(t + 1) * 128, :], "x2s", [(t, t + 1)]))
                for j, din_ in enumerate((d1i, d2i)):
                    S.op("pool", lambda e, xr_=xr_, din_=din_, t=t: e.indirect_dma_start(
                        out=d_xin[:, :], out_offset=bass.IndirectOffsetOnAxis(ap=din_.v(slice(t, t + 1)).ap, axis=0),
                        in_=xr_.v().ap, in_offset=None),
                        reads=[xr_.v(), din_.v(slice(t, t + 1))], writes=[View(None, "xin", [(2 * t + j, 2 * t + j + 1)])], dma=True)
            if stop <= 4.9:
                return
            _bc = {}

            def _bc_reg(e):
                if "r" not in _bc:
                    _bc["r"] = e.to_reg(N_EXP * 128 - 1)
                return _bc["r"]
            xin_all = View(None, "xin", [(0, 2 * NT)])
            NQ = N_SLOTS * SUBS

            order = list(range(NWS))
            _ip, _io = NWS, N_EXP
            for ch_ in "PPOPPOPPOPO" * 4:
                if ch_ == "P":
                    order.append(_ip)
                    _ip += 1
                else:
                    order.append(_io)
                    _io += 1
            assert sorted(order) == list(range(N_SLOTS)), order

            def load_w(pos_):
                s_ = order[pos_]
                if s_ < N_EXP:
                    wg_b, wu_b, wd_b = wexp[pos_ % NWS]
                    dma("pool", wg_b.v(), D_(d_wg[s_]))
                    dma("pool", wu_b.v(), D_(d_wu[s_]))
                    dma("pool", wd_b.v(), D_(d_wd[s_]))
                    return
                wg2, wu2, wd2 = wexp2[pos_ % NWS]
                s_ = s_ - N_EXP
                for (w2_, drows) in ((wg2, d_wg_rows), (wu2, d_wu_rows), (wd2, d_wd_rows)):
                    S.op("pool", lambda e, w2_=w2_, drows=drows, s_=s_: e.indirect_dma_start(
                        out=w2_.v().ap, out_offset=None, in_=drows,
                        in_offset=bass.IndirectOffsetOnAxis(ap=widx_i.v(slice(s_, s_ + 1)).ap, axis=0),
                        bounds_check=_bc_reg(e), oob_is_err=False),
                        reads=[widx_i.v(slice(s_, s_ + 1))], writes=[w2_.v()], dma=True)

            def stage_L(qs):
                q = order[qs // SUBS] * SUBS + qs % SUBS
                r0 = q * 128
                xb_ = xb[qs % 4]
                S.op("act", lambda e, xb_=xb_, r0=r0: e.dma_start(out=xb_.v().ap, in_=d_xin[r0:r0 + 128, :]),
                     reads=[xin_all], writes=[xb_.v()], dma=True)

            def stage_T(qs):
                xb_ = xb[qs % 4]
                xbT_ = xbT[qs % 4]
                for hb in range(2):
                    bt = next_bank()
                    ptb = psbuf(bt, [4, 256], BF16)
                    for kk in range(4):
                        k = hb * 4 + kk
                        tr(ptb.v(kk, slice(0, 128)), xb_.v(slice(k * 128, (k + 1) * 128)), ident_b.v())
                    cp("act" if hb == 0 else "dve", xbT_.v(slice(hb * 4, hb * 4 + 4)), ptb.v(slice(0, 4), slice(0, 128)))

            def stage_G(qs):
                wg_b, wu_b, wd_b = wexp[(qs // SUBS) % NWS]
                xbT_ = xbT[qs % 4]
                hT_ = hTs[qs % 4]
                bg, bu = next_bank(), next_bank()
                for f in range(4):
                    fs = slice(f * 128, (f + 1) * 128)
                    for k in range(8):
                        mm(PSF[bg].v(fs), wg_b.v(k, fs), xbT_.v(k), k == 0, k == 7)
                for f in range(4):
                    fs = slice(f * 128, (f + 1) * 128)
                    for k in range(8):
                        mm(PSF[bu].v(fs), wu_b.v(k, fs), xbT_.v(k), k == 0, k == 7)
                s_l = sl[qs % 2]
                act(s_l.v(), PSF[bg].v(), AF.Silu)
                tt("dve", hT_.v(), PSF[bu].v(), s_l.v(), ALU.mult)

            def stage_D(qs):
                q = order[qs // SUBS] * SUBS + qs % SUBS
                wg_b, wu_b, wd_b = wexp[(qs // SUBS) % NWS]
                hT4 = hTs4[qs % 4]
                yb_ = ybs[qs % 4]
                r0 = q * 128
                for hf in range(2):
                    b = next_bank()
                    for f in range(4):
                        mm(PSF[b].v(), hT4.v(f), wd_b.v(f, slice(hf * 512, (hf + 1) * 512)), f == 0, f == 3)
                    cp("act" if hf == 0 else "dve", yb_.v(slice(hf * 512, (hf + 1) * 512)), PSF[b].v())
                S.op("sp", lambda e, yb_=yb_, r0=r0: e.dma_start(out=d_yb[r0:r0 + 128, :], in_=yb_.v().ap),
                     reads=[yb_.v()], writes=[View(None, "yb", [(q, q + 1)])], dma=True)

            next_w = NWS
            XB_AHEAD = 2
            for it in range(NQ + 3):
                while next_w < N_SLOTS and (next_w < NWS or it >= (next_w - NWS) * SUBS + SUBS + 3):
                    load_w(next_w)
                    next_w += 1
                if it == 0:
                    for q_ in range(min(XB_AHEAD, NQ)):
                        stage_L(q_)
                if it + XB_AHEAD < NQ:
                    stage_L(it + XB_AHEAD)
                if it < NQ:
                    stage_T(it)
                if 3 <= it < NQ + 3:
                    stage_D(it - 3)
                if 1 <= it <= NQ:
                    stage_G(it - 1)
            assert next_w == N_SLOTS

            if stop <= 5:
                return
            yb_all = View(None, "yb", [(0, N_SLOTS * SUBS)])
            dma("sp", ln2g.v(), D_(d_ln2g))
            dma("sp", ln2b.v(), D_(d_ln2b))
            X2_AHEAD = 3

            def load_x2r(t):
                S.op("act", lambda e, t=t: e.dma_start(out=x2rb[t % 4].v().ap, in_=d_x2s[t * 128:(t + 1) * 128, :]),
                     reads=[View(None, "x2s", [(t, t + 1)])], writes=[x2rb[t % 4].v()], dma=True)
            for t in range(min(X2_AHEAD, NT)):
                load_x2r(t)
            for t in range(NT):
                xr_ = x2rb[t % 4]
                ab = a2[t % 2]
                g1, g2 = rg1[t % 2], rg2[t % 2]
                if t + X2_AHEAD < NT:
                    load_x2r(t + X2_AHEAD)
                for (gb, din_) in ((g1, d1i), (g2, d2i)):
                    S.op("pool", lambda e, gb=gb, din_=din_, t=t: e.indirect_dma_start(
                        out=gb.v().ap, out_offset=None, in_=d_yb[:, :],
                        in_offset=bass.IndirectOffsetOnAxis(ap=din_.v(slice(t, t + 1)).ap, axis=0)),
                        reads=[yb_all, din_.v(slice(t, t + 1))], writes=[gb.v()], dma=True)
                act(g1.v(), g1.v(), AF.Identity, scale=w12.v(0, slice(t, t + 1)))
                stt(g1.v(), g2.v(), w12.v(1, slice(t, t + 1)), g1.v(), ALU.mult, ALU.add)
                stt(ab.v(), xr_.v(), ALPHA, g1.v(), ALU.mult, ALU.add)
                layer_norm(ab, stats2, mv2, rstd2, ln2g, ln2b, ab, mul_eng="dve", nbias_b=nbias2)
                dma("sp", View(d_out[t * 128:(t + 1) * 128, :], "out", [(t, t + 1)]), ab.v())

        body()
        for (dv, bv_) in dbg_list:
            dma("sp", View(dv, "dbgout", [(0, 1)]), bv_)
        S.emit(es)
    return nc


def _prep_shared(inp, with_experts=True):
    f = np.float32
    w_in = np.asarray(inp["w_in"], f)[0]
    b_in = np.asarray(inp["b_in"], f)[0]
    cols = []
    for g in range(4):
        cols.append(np.arange(g * 128, (g + 1) * 128))
    d = np.arange(64)
    sw = (d + 32) % 64
    for c in range(4):
        cols.append(np.concatenate([512 + c * 64 + d, 512 + (4 + c) * 64 + d]))
    for c in range(4):
        cols.append(np.concatenate([512 + c * 64 + sw, 512 + (4 + c) * 64 + sw]))
    cols.append(np.concatenate([1024 + d, 1024 + 64 + d]))
    cols.append(np.concatenate([1024 + sw, 1024 + 64 + sw]))
    cols.append(np.arange(1152, 1280))
    for m in range(16):
        cols.append(np.arange(1280 + m * 128, 1280 + (m + 1) * 128))
    cols = np.stack(cols)
    assert cols.shape == (N_INCH, 128)
    wsel = w_in[:, cols]
    w_in_r = np.ascontiguousarray(wsel.reshape(8, 128, N_INCH, 128).transpose(2, 1, 0, 3))
    b_in_r = np.ascontiguousarray(b_in[cols].T)
    bv_bc = np.ascontiguousarray(np.broadcast_to(b_in[1152:1280][None, :], (128, 128)))
    pos = np.arange(S_LEN, dtype=f)
    inv = (f(10000.0) ** (-np.arange(0, 64, 2, dtype=f) / f(64))).astype(f)
    ang = (pos[:, None] * inv[None, :]).astype(f)
    cos = np.cos(ang).astype(f)
    sin = np.sin(ang).astype(f)
    p = np.arange(128)
    fi = (p % 64) % 32
    sign = np.where((p % 64) < 32, -1.0, 1.0).astype(f)
    cosT = np.ascontiguousarray(cos[:, fi].T)
    sinT = np.ascontiguousarray((sin[:, fi] * sign[None, :]).T.astype(f))
    ec = np.ones((128, 4, 16), f)
    tpos = np.concatenate([np.arange(8), np.arange(S_LEN - 8, S_LEN)])
    for g, w in enumerate((2, 4, 8, 16)):
        lo = np.clip(tpos - w // 2, 0, S_LEN)
        hi = np.clip(tpos - w // 2 + w, 0, S_LEN)
        ec[:, g, :] = (f(w) / (hi - lo).astype(f))[None, :]
    rep = lambda v, n=128: np.ascontiguousarray(np.broadcast_to(np.asarray(v, f)[None, :], (n, len(v))))
    sh = {
        "w_in_r": w_in_r, "b_in_r": b_in_r, "bv_bc": bv_bc, "cosT": cosT, "sinT": sinT, "ec": ec,
        "wmix_r": np.ascontiguousarray(np.asarray(inp["w_pool_mix"], f)[0].transpose(1, 0, 2)),
        "bmix_r": np.ascontiguousarray(np.asarray(inp["b_pool_mix"], f)[0].T),
        "pscale_r": np.ascontiguousarray(np.asarray(inp["pool_scale"], f)[0].reshape(4, 128).T),
        "sink_bc": rep(np.asarray(inp["attn_sink"], f)[0]),
        "wpb_r": np.ascontiguousarray(np.asarray(inp["w_pool_br"], f)[0].reshape(4, 128, D).transpose(1, 0, 2)),
        "wab_r": np.ascontiguousarray(np.asarray(inp["w_attn_br"], f)[0].reshape(4, 128, D).transpose(1, 0, 2)),
        "wo_r": np.ascontiguousarray(np.asarray(inp["w_o"], f)[0].reshape(8, 128, D).transpose(1, 0, 2)),
        "ln1g_bc": rep(np.asarray(inp["ln1_g"], f)[0]), "ln1b_bc": rep(np.asarray(inp["ln1_b"], f)[0]),
        "ln2g_bc": rep(np.asarray(inp["ln2_g"], f)[0]), "ln2b_bc": rep(np.asarray(inp["ln2_b"], f)[0]),
        "wr_r": np.ascontiguousarray(np.concatenate([np.asarray(inp["w_router_group"], f)[0],
                                                     np.asarray(inp["w_router_expert"], f)[0]], axis=1)
                                     .reshape(8, 128, 36).transpose(1, 0, 2)),
        "br_bc": rep(np.concatenate([np.asarray(inp["b_router_group"], f)[0], np.asarray(inp["b_router_expert"], f)[0]])),
    }
    if with_experts:
        sh.update({
        "wg_r": np.ascontiguousarray(np.asarray(inp["w_gate"], f)[0].reshape(N_EXP, 8, 128, 512).transpose(0, 2, 1, 3)),
        "wu_r": np.ascontiguousarray(np.asarray(inp["w_up"], f)[0].reshape(N_EXP, 8, 128, 512).transpose(0, 2, 1, 3)),
        "wd_r": np.ascontiguousarray(np.asarray(inp["w_down"], f)[0].reshape(N_EXP, 4, 128, D).transpose(0, 2, 1, 3)),
        })
    return sh


_NC_CACHE = {}


def run_debug(inputs, stop, n_cores=N_CORES):
    x = np.asarray(inputs["x"], np.float32)
    sh = _prep_shared(inputs, with_experts=(stop >= 5))
    nc = build_program(stop=stop, dbg=True)
    in_maps = []
    for c in range(n_cores):
        m = dict(sh)
        m["x_tok"] = np.ascontiguousarray(x[c])
        m["xT"] = np.ascontiguousarray(x[c].T)
        in_maps.append(m)
    res = run_bass_kernel_spmd(nc, in_maps, core_ids=list(range(n_cores)))
    return res.results


def kernel(**inputs):
    x = np.asarray(inputs["x"], np.float32)
    sh = _prep_shared(inputs)
    if "nc" not in _NC_CACHE:
        _NC_CACHE["nc"] = build_program()
    nc = _NC_CACHE["nc"]
    in_maps = []
    for c in range(N_CORES):
        m = dict(sh)
        m["x_tok"] = np.ascontiguousarray(x[c])
        m["xT"] = np.ascontiguousarray(x[c].T)
        in_maps.append(m)
    res = run_bass_kernel_spmd(nc, in_maps, core_ids=list(range(N_CORES)))
    out = np.stack([np.asarray(res.results[c]["out"], np.float32) for c in range(N_CORES)], axis=0)
    return out
```

```python
import contextlib
import numpy as np
import concourse.bass as bass
import concourse.mybir as mybir
from concourse.bass_utils import run_bass_kernel_spmd

F32 = mybir.dt.float32
BF16 = mybir.dt.bfloat16
AF = mybir.ActivationFunctionType
ALU = mybir.AluOpType
AX = mybir.AxisListType

S_LEN = 2048
D = 1024
NT = 16
NS = 4
N_EXP = 32
ALPHA = 2.0 ** 0.25
LN_EPS = 1e-5
N_CORES = 8
N_INCH = 31
SLOT_B = 256
SLOT_SHIFT = 8
SUBS = SLOT_B // 128
N_OVF = (2 * S_LEN) // SLOT_B
N_SLOTS = N_EXP + N_OVF
I32 = mybir.dt.int32

COMPUTE = ("pe", "act", "dve", "pool")
ENGS = ("pe", "act", "dve", "pool", "sp")
PAGE = 2048


class View:
    __slots__ = ("ap", "arena", "ivals")

    def __init__(self, ap, arena, ivals):
        self.ap = ap
        self.arena = arena
        self.ivals = ivals


def _ivals(shape, esize, base, idx):
    nd = len(shape)
    rngs = []
    for d in range(nd):
        if d < len(idx):
            i = idx[d]
            if isinstance(i, int):
                rngs.append((i, i + 1))
            else:
                a = 0 if i.start is None else i.start
                b = shape[d] if i.stop is None else i.stop
                rngs.append((a, b))
        else:
            rngs.append((0, shape[d]))
    strides = [0] * nd
    s = esize
    for d in range(nd - 1, -1, -1):
        strides[d] = s
        s *= shape[d]
    tail = nd
    while tail > 0 and rngs[tail - 1] == (0, shape[tail - 1]):
        tail -= 1
    out = []
    if tail == 0:
        return [(base, base + s)]
    lead = rngs[: tail - 1]
    a, b = rngs[tail - 1]
    blk_lo = a * strides[tail - 1]
    blk_hi = b * strides[tail - 1]
    cnt = 1
    for (x, y) in lead:
        cnt *= (y - x)
    if cnt > 64:
        lo = base + sum(r[0] * strides[d] for d, r in enumerate(lead)) + blk_lo
        hi = base + sum((r[1] - 1) * strides[d] for d, r in enumerate(lead)) + blk_hi
        return [(lo, hi)]

    def rec(d, off):
        if d == tail - 1:
            out.append((base + off + blk_lo, base + off + blk_hi))
            return
        for i in range(lead[d][0], lead[d][1]):
            rec(d + 1, off + i * strides[d])
    rec(0, 0)
    return out


class Buf:
    def __init__(self, arena_name, ap_full, shape, esize, base_bytes):
        self.arena = arena_name
        self.ap_full = ap_full
        self.shape = tuple(shape)
        self.esize = esize
        self.base = base_bytes

    def v(self, *idx, p=None):
        full = (slice(None) if p is None else p,) + tuple(idx)
        ap = self.ap_full[full]
        if self.arena == "ps":
            return View(ap, self.arena, [(self.base, self.base + 2048)])
        return View(ap, self.arena, _ivals(self.shape, self.esize, self.base, idx))


class Sched:
    def __init__(self, nc, n_dma_sems=32):
        self.nc = nc
        self.streams = {e: [] for e in ENGS}
        self.cnt = {e: 0 for e in COMPUTE}
        self.pages = {}
        self.waited = {}
        self.n_dma_sems = n_dma_sems
        self.dma_issued = 0
        self.dma_slot_val = [0] * n_dma_sems

    def _touch(self, view, is_write, deps, own_sem=None):
        ps = view.arena == "ps"
        for (lo, hi) in view.ivals:
            for pg in range(lo // PAGE, (hi - 1) // PAGE + 1):
                d = self.pages.get((view.arena, pg))
                if not d:
                    continue
                for (rlo, rhi, sem, kind), val in d.items():
                    if rlo < hi and lo < rhi and (is_write or kind == "w" or (ps and sem != own_sem)):
                        deps.append((sem, val))

    def _record(self, view, is_write, token):
        sem, val = token
        kind = "w" if is_write else "r"
        for (lo, hi) in view.ivals:
            for pg in range(lo // PAGE, (hi - 1) // PAGE + 1):
                d = self.pages.setdefault((view.arena, pg), {})
                if is_write:
                    dead = [k for k in d if k[0] >= lo and k[1] <= hi]
                    for k in dead:
                        del d[k]
                d[(lo, hi, sem, kind)] = val

    def op(self, eng, fn, reads=(), writes=(), dma=False):
        deps = []
        own = None if dma else (eng,)
        for v in reads:
            self._touch(v, False, deps, own)
        for v in writes:
            self._touch(v, True, deps, own)
        if dma:
            slot = self.dma_issued % self.n_dma_sems
            self.dma_issued += 1
            prev = self.dma_slot_val[slot]
            if prev > 0:
                deps.append((("dma", slot), prev))
            self.dma_slot_val[slot] = prev + 16
            token = (("dma", slot), prev + 16)
            inc = 16
        else:
            self.cnt[eng] += 1
            token = ((eng,), self.cnt[eng])
            inc = 1
        need = {}
        for sem, val in deps:
            if (not dma) and eng == "pe" and sem == ("pe",):
                continue
            if need.get(sem, 0) < val:
                need[sem] = val
        waits = []
        for sem, val in need.items():
            if self.waited.get((eng, sem), 0) >= val:
                continue
            self.waited[(eng, sem)] = val
            waits.append((sem, val))
        self.streams[eng].append((waits, fn, token, inc))
        for v in reads:
            self._record(v, False, token)
        for v in writes:
            self._record(v, True, token)
        return token

    def emit(self, es, final_wait_eng="sp"):
        nc = self.nc
        sems = {}
        for e in COMPUTE:
            sems[(e,)] = es.enter_context(nc.semaphore("s_" + e))
        for i in range(self.n_dma_sems):
            sems[("dma", i)] = es.enter_context(nc.semaphore("s_dma%d" % i))
        block = es.enter_context(nc.Block())
        finals = []
        for e in COMPUTE:
            if self.cnt[e] > 0:
                finals.append(((e,), self.cnt[e]))
        for i in range(self.n_dma_sems):
            if self.dma_slot_val[i] > 0:
                finals.append((("dma", i), self.dma_slot_val[i]))

        def make(engname):
            stream = self.streams[engname]

            def body(eng):
                for waits, fn, token, inc in stream:
                    for sem, val in waits:
                        eng.wait_ge(sems[sem], val)
                    ins = fn(eng)
                    ins.then_inc(sems[token[0]], inc)
                if engname == final_wait_eng:
                    for sem, val in finals:
                        eng.wait_ge(sems[sem], val)
            return body

        block.tensor(make("pe"))
        block.scalar(make("act"))
        block.vector(make("dve"))
        block.gpsimd(make("pool"))
        block.sync(make("sp"))


ARENA_WORDS = 51200


def build_program(stop=99, dbg=False):
    nc = bass.Bass("TRN2", target_bir_lowering=False)
    dbg_list = []

    def din(name, shape, dt=F32):
        return nc.dram_tensor(name, list(shape), dt, kind="ExternalInput").ap()

    d_xT = din("xT", [D, S_LEN])
    d_x = din("x_tok", [S_LEN, D])
    d_win = din("w_in_r", [N_INCH, 128, 8, 128])
    d_bin = din("b_in_r", [128, N_INCH])
    d_bv = din("bv_bc", [128, 128])
    d_cos = din("cosT", [128, S_LEN])
    d_sin = din("sinT", [128, S_LEN])
    d_ec = din("ec", [128, 4, 16])
    d_wmix = din("wmix_r", [128, 4, 128])
    d_bmix = din("bmix_r", [128, 4])
    d_pscale = din("pscale_r", [128, 4])
    d_sink = din("sink_bc", [128, 8])
    d_wpb = din("wpb_r", [128, 4, D])
    d_wab = din("wab_r", [128, 4, D])
    d_wo = din("wo_r", [128, 8, D])
    d_ln1g = din("ln1g_bc", [128, D])
    d_ln1b = din("ln1b_bc", [128, D])
    d_ln2g = din("ln2g_bc", [128, D])
    d_ln2b = din("ln2b_bc", [128, D])
    d_wr = din("wr_r", [128, 8, 36])
    d_br = din("br_bc", [128, 36])
    if stop >= 5:
        d_wg = din("wg_r", [N_EXP, 128, 8, 512])
        d_wu = din("wu_r", [N_EXP, 128, 8, 512])
        d_wd = din("wd_r", [N_EXP, 128, 4, D])
        d_wg_rows = d_wg.rearrange("e p k m -> (e p) (k m)")
        d_wu_rows = d_wu.rearrange("e p k m -> (e p) (k m)")
        d_wd_rows = d_wd.rearrange("e p k m -> (e p) (k m)")
    d_xin = nc.dram_tensor("xin_scratch", [N_SLOTS * SLOT_B, D], BF16, kind="Internal").ap()
    d_yb = nc.dram_tensor("yb_scratch", [N_SLOTS * SLOT_B, D], F32, kind="Internal").ap()
    d_out = nc.dram_tensor("out", [S_LEN, D], F32, kind="ExternalOutput").ap()
    d_x2s = nc.dram_tensor("x2_scratch", [S_LEN, D], F32, kind=("ExternalOutput" if dbg else "Internal")).ap()

    es = contextlib.ExitStack()
    with es:
        arena = es.enter_context(nc.sbuf_tensor("arena", [128, ARENA_WORDS], F32))
        banks_t = [es.enter_context(nc.psum_tensor("ps%d" % i, [128, 512], F32)) for i in range(8)]
        S = Sched(nc)

        def buf(off, shape, dt=F32):
            n = int(np.prod(shape))
            if dt == F32:
                words = n
                ap = arena[:, off:off + words]
                esize = 4
            elif dt == I32:
                words = n
                ap = arena[:, off:off + words].bitcast(I32)
                esize = 4
            else:
                assert n % 2 == 0
                words = n // 2
                ap = arena[:, off:off + words].bitcast(BF16)
                esize = 2
            if len(shape) > 1:
                names = " ".join("d%d" % i for i in range(len(shape)))
                kw = {"d%d" % i: shape[i] for i in range(1, len(shape))}
                ap = ap.rearrange("p (%s) -> p %s" % (names, names), **kw)
            return Buf("sb", ap, shape, esize, off * 4), off + words

        def psbuf(i, shape, dt=F32):
            n_ = int(np.prod(shape))
            if dt == F32:
                ap = banks_t[i][:, 0:n_]
                esize = 4
            else:
                ap = banks_t[i][:].bitcast(BF16)[:, 0:n_]
                esize = 2
            if len(shape) > 1:
                names = " ".join("d%d" % j for j in range(len(shape)))
                kw = {"d%d" % j: shape[j] for j in range(1, len(shape))}
                ap = ap.rearrange("p (%s) -> p %s" % (names, names), **kw)
            return Buf("ps", ap, shape, esize, i * 2048)

        PSF = [psbuf(i, [512]) for i in range(8)]
        PSB = [psbuf(i, [1024], BF16) for i in range(8)]
        ps_rr = [0]

        def next_bank():
            i = ps_rr[0] % 8
            ps_rr[0] += 1
            return i

        def dview(ap, name, lo, hi):
            return View(ap, name, [(lo, hi)])

        def mm(out, lhsT, rhs, start, stop):
            S.op("pe", lambda e: e.matmul(out.ap, lhsT.ap, rhs.ap, start=start, stop=stop),
                 reads=[lhsT, rhs], writes=[out])

        def tr(out, in_, ident):
            S.op("pe", lambda e: e.transpose(out.ap, in_.ap, ident.ap), reads=[in_, ident], writes=[out])

        def act(out, in_, func, bias=0.0, scale=1.0, accum=None, eng="act"):
            rd = [in_]
            b = bias
            s = scale
            if isinstance(bias, View):
                rd.append(bias)
                b = bias.ap
            if isinstance(scale, View):
                rd.append(scale)
                s = scale.ap
            wr = [out]
            if accum is not None:
                wr.append(accum)
                S.op(eng, lambda e: e.activation(out=out.ap, in_=in_.ap, func=func, bias=b, scale=s,
                                                 accum_out=accum.ap), reads=rd, writes=wr)
            else:
                S.op(eng, lambda e: e.activation(out=out.ap, in_=in_.ap, func=func, bias=b, scale=s),
                     reads=rd, writes=wr)

        def tt(eng, out, in0, in1, op):
            S.op(eng, lambda e: e.tensor_tensor(out=out.ap, in0=in0.ap, in1=in1.ap, op=op),
                 reads=[in0, in1], writes=[out])

        def ts(eng, out, in0, s1, op0, s2=None, op1=None):
            rd = [in0]
            a1 = s1
            a2 = s2
            if isinstance(s1, View):
                rd.append(s1)
                a1 = s1.ap
            if isinstance(s2, View):
                rd.append(s2)
                a2 = s2.ap
            if op1 is None:
                S.op(eng, lambda e: e.tensor_scalar(out=out.ap, in0=in0.ap, scalar1=a1, scalar2=None, op0=op0),
                     reads=rd, writes=[out])
            else:
                S.op(eng, lambda e: e.tensor_scalar(out=out.ap, in0=in0.ap, scalar1=a1, scalar2=a2, op0=op0, op1=op1),
                     reads=rd, writes=[out])

        def stt(out, in0, scalar, in1, op0, op1):
            rd = [in0, in1]
            sc = scalar
            if isinstance(scalar, View):
                rd.append(scalar)
                sc = scalar.ap
            S.op("dve", lambda e: e.scalar_tensor_tensor(out=out.ap, in0=in0.ap, scalar=sc, in1=in1.ap, op0=op0, op1=op1),
                 reads=rd, writes=[out])

        def cp(eng, out, in_):
            if eng == "act":
                S.op(eng, lambda e: e.copy(out=out.ap, in_=in_.ap), reads=[in_], writes=[out])
            else:
                S.op(eng, lambda e: e.tensor_copy(out=out.ap, in_=in_.ap), reads=[in_], writes=[out])

        def memset(eng, out, val):
            S.op(eng, lambda e: e.memset(out.ap, val), writes=[out])

        def dma(eng, out, in_):
            S.op(eng, lambda e: e.dma_start(out=out.ap, in_=in_.ap), reads=[in_], writes=[out], dma=True)

        A0, B0, C0, D0, E0, F0 = 0, 16384, 28672, 36864, 45056, 50176
        o = F0
        ident_f, o = buf(o, [128])
        ident_b, o = buf(o, [128], BF16)
        wmix, o = buf(o, [4, 128], BF16)
        wr_f, o = buf(o, [8, 36])
        b_in, o = buf(o, [N_INCH])
        bmix, o = buf(o, [4])
        pscale, o = buf(o, [4])
        bms, o = buf(o, [4])
        esink, o = buf(o, [8])
        br_bc, o = buf(o, [36])
        ec, o = buf(o, [4, 16])
        den, o = buf(o, [4])
        rden, o = buf(o, [4])
        assert o <= ARENA_WORDS, o
        xT, _ = buf(D0, [8, S_LEN], BF16)
        mixT, _ = buf(C0, [4, S_LEN], BF16)
        oT, _ = buf(C0 + 4096, [4, S_LEN], BF16)
        hT = [buf(C0 + 1024 * i, [4, 512], BF16)[0] for i in range(8)]
        o = B0
        wch = []
        for i in range(4):
            b_, o = buf(o, [8, 128], BF16)
            wch.append(b_)
        wpb, o = buf(o, [4, D], BF16)
        wab, o = buf(o, [4, D], BF16)
        wo, o = buf(o, [8, D], BF16)
        mT, o = buf(o, [8, 512], BF16)
        assert o == C0
        wexp = []
        o = B0
        for i in range(2):
            g_, o = buf(o, [8, 512], BF16)
            u_, o = buf(o, [8, 512], BF16)
            d_, o = buf(o, [4, D], BF16)
            wexp.append((g_, u_, d_))
        o = A0
        PADW = S_LEN + 16
        uT, o = buf(o, [PADW])
        sA, o = buf(o, [PADW])
        sB, o = buf(o, [PADW])
        cosT, o = buf(o, [S_LEN])
        sinT, o = buf(o, [S_LEN])
        qT, o = buf(o, [4, S_LEN], BF16)
        kT, o = buf(o, [S_LEN], BF16)
        otok = []
        for i in range(2):
            b_, o = buf(o, [512], BF16)
            otok.append(b_)
        assert o <= B0, o
        o = A0
        sg0, o = buf(o, [512])
        sg1, o = buf(o, [512])
        t0b, o = buf(o, [512])
        t1b, o = buf(o, [512])
        xtok = []
        abuf = []
        for i in range(4):
            b_, o = buf(o, [D])
            xtok.append(b_)
        for i in range(2):
            b_, o = buf(o, [D])
            abuf.append(b_)
        x2Tf, o = buf(o, [8, 128])
        ln1g, o = buf(o, [D])
        ln1b, o = buf(o, [D])
        stats, o = buf(o, [2, 6])
        mv, o = buf(o, [2])
        rstd, o = buf(o, [1])
        lg, o = buf(o, [36])
        rt, o = buf(o, [64])
        assert o <= B0, o
        o = E0 + 1536
        OH1f, o = buf(o, [NT, 32])
        OH2f, o = buf(o, [NT, 32])
        w12, o = buf(o, [2, NT])
        assert o <= F0, o
        o = A0
        prefix, o = buf(o, [NT, 32])
        slotpos, o = buf(o, [NT, 32])
        tmp512, o = buf(o, [NT, 32])
        ind, o = buf(o, [NT, 32], BF16)
        ones_bf, o = buf(o, [128], BF16)
        stri_bf, o = buf(o, [128], BF16)
        cnt, o = buf(o, [32])
        cnt_i, o = buf(o, [32], I32)
        nb_i, o = buf(o, [32], I32)
        nb_f, o = buf(o, [32])
        pendb, o = buf(o, [32])
        pstart, o = buf(o, [32])
        zeros32, o = buf(o, [32])
        junk32, o = buf(o, [32])
        es_f, o = buf(o, [N_SLOTS])
        widx_f, o = buf(o, [N_SLOTS])
        widx_i, o = buf(o, [N_SLOTS], I32)
        d1f, o = buf(o, [NT])
        d2f, o = buf(o, [NT])
        d1i, o = buf(o, [NT], I32)
        d2i, o = buf(o, [NT], I32)
        cm_f, o = buf(o, [32])
        basep, o = buf(o, [32])
        basep_i, o = buf(o, [32], I32)
        pidx, o = buf(o, [1])
        pidx_i, o = buf(o, [1], I32)
        o += (-o) % 2
        o_rg = o
        rg1, rg2 = [], []
        for i in range(2):
            b_, o_rg = buf(o_rg, [D])
            rg1.append(b_)
            b_, o_rg = buf(o_rg, [D])
            rg2.append(b_)
        xr = []
        for i in range(2):
            b_, o = buf(o, [D])
            xr.append(b_)
        xb, xbT = [], []
        for i in range(4):
            b_, o = buf(o, [D], BF16)
            xb.append(b_)
        for i in range(4):
            b_, o = buf(o, [8, 128], BF16)
            xbT.append(b_)
        assert o <= A0 + 8704, o
        o = C0
        hTs, hTs4, ybs = [], [], []
        for i in range(4):
            h4, _ = buf(o, [4, 128], BF16)
            b_, o = buf(o, [512], BF16)
            hTs.append(b_)
            hTs4.append(h4)
        for i in range(4):
            b_, o = buf(o, [D])
            ybs.append(b_)
        assert o <= D0, o
        wexp2 = []
        o = B0
        for i in range(2):
            g_, o = buf(o, [4096], BF16)
            u_, o = buf(o, [4096], BF16)
            d_, o = buf(o, [4096], BF16)
            wexp2.append((g_, u_, d_))
        o = A0 + 8704
        g_, o = buf(o, [8, 512], BF16)
        u_, o = buf(o, [8, 512], BF16)
        d_, o = buf(o, [4, D], BF16)
        wexp.append((g_, u_, d_))
        assert o <= B0, o
        o = A0 + 8704
        g_, o = buf(o, [4096], BF16)
        u_, o = buf(o, [4096], BF16)
        d_, o = buf(o, [4096], BF16)
        wexp2.append((g_, u_, d_))
        o = D0
        g_, o = buf(o, [8, 512], BF16)
        u_, o = buf(o, [8, 512], BF16)
        d_, o = buf(o, [4, D], BF16)
        wexp.append((g_, u_, d_))
        assert o <= E0, o
        o = D0
        g_, o = buf(o, [4096], BF16)
        u_, o = buf(o, [4096], BF16)
        d_, o = buf(o, [4096], BF16)
        wexp2.append((g_, u_, d_))
        NWS = 4
        wexp = [wexp[0], wexp[1], wexp[3], wexp[2]]
        wexp2 = [wexp2[0], wexp2[1], wexp2[3], wexp2[2]]
        xr.append(buf(C0 + 1024, [D])[0])
        xr.append(buf(C0 + 2048, [D])[0])
        o = E0
        vaug, o = buf(o, [NT, 2, 65], BF16)
        pT = []
        for i in range(2):
            b_, o = buf(o, [3, 512], BF16)
            pT.append(b_)
        pooled, o = buf(o, [S_LEN], BF16)
        rtmp1, o = buf(o, [512])
        rtmp2, o = buf(o, [512])
        bv_bc, o = buf(o, [128])
        assert o <= F0, o
        o = E0
        sl = []
        for i in range(2):
            b_, o = buf(o, [512])
            sl.append(b_)
        coef, o = buf(o, [NT, 32])
        assert o <= F0, o
        o = B0
        ln2g, o = buf(o, [D])
        ln2b, o = buf(o, [D])
        x2rb = []
        a2 = []
        for i in range(4):
            b_, o = buf(o, [D])
            x2rb.append(b_)
        for i in range(2):
            b_, o = buf(o, [D])
            a2.append(b_)
        stats2, o = buf(o, [2, 6])
        mv2, o = buf(o, [2])
        rstd2, o = buf(o, [1])
        nbias2, o = buf(o, [1])
        assert o <= C0

        if dbg:
            def dout(name, shape, dt):
                return nc.dram_tensor(name, list(shape), dt, kind="ExternalOutput").ap()
            if 1 <= stop <= 4:
                dbg_list.append((dout("dbg_mixT", [128, 4, S_LEN], BF16), mixT.v()))
            if 2 <= stop <= 3:
                dbg_list.append((dout("dbg_qT", [128, 4, S_LEN], BF16), qT.v()))
                dbg_list.append((dout("dbg_kT", [128, S_LEN], BF16), kT.v()))
                dbg_list.append((dout("dbg_vaug", [128, NT, 2, 65], BF16), vaug.v()))
            if 3 <= stop <= 4:
                dbg_list.append((dout("dbg_oT", [128, 4, S_LEN], BF16), oT.v()))
            if stop >= 4:
                dbg_list.append((dout("dbg_coef", [128, NT, 32], F32), coef.v()))
            if stop == 4:
                dbg_list.append((dout("dbg_x2T", [128, 8, S_LEN], BF16), xT.v()))
            if stop >= 4.5:
                dbg_list.append((dout("dbg_d1", [128, NT], F32), d1f.v()))
                dbg_list.append((dout("dbg_d2", [128, NT], F32), d2f.v()))
                dbg_list.append((dout("dbg_es", [128, N_SLOTS], F32), es_f.v()))
                dbg_list.append((dout("dbg_w12", [128, 2, NT], F32), w12.v()))

        def D_(ap, name="dram_in", lo=0, hi=1):
            return View(ap, name, [(lo, hi)])

        def body():
            dma("sp", b_in.v(), D_(d_bin))
            dma("sp", bmix.v(), D_(d_bmix))
            dma("sp", pscale.v(), D_(d_pscale))
            dma("sp", esink.v(), D_(d_sink))
            dma("sp", br_bc.v(), D_(d_br))
            dma("sp", ec.v(), D_(d_ec))
            dma("sp", wr_f.v(), D_(d_wr))
            dma("sp", bv_bc.v(), D_(d_bv))
            dma("sp", cosT.v(), D_(d_cos))
            dma("sp", sinT.v(), D_(d_sin))
            dma("pool", wmix.v(), D_(d_wmix))
            for k in range(8):
                dma("pool", xT.v(k), D_(d_xT[k * 128:(k + 1) * 128, :]))
            memset("pool", ident_f.v(), 0.0)
            S.op("pool", lambda e: e.affine_select(out=ident_f.v().ap, in_=ident_f.v().ap, pattern=[[-1, 128]],
                                                   compare_op=ALU.not_equal, fill=1.0, base=0, channel_multiplier=1),
                 reads=[ident_f.v()], writes=[ident_f.v()])
            cp("dve", ident_b.v(), ident_f.v())
            tt("dve", bms.v(), bmix.v(), pscale.v(), ALU.mult)
            act(esink.v(), esink.v(), AF.Exp)
            memset("pool", vaug.v(), 1.0)
            memset("dve", uT.v(), 0.0)

            wch_seq = ([0, 1, 2, 3, 4, 8, 5, 9, 6, 10, 7, 11, 12, 13, 14]
                       + [c for _n in range(NS) for m_ in range(8) for c in (15 + m_, 23 + m_)])
            wch_pos = [0]
            wch_issued = [0]
            WCH_AHEAD = 2

            def load_wchunk(ch):
                pos = wch_pos[0]
                assert wch_seq[pos] == ch, (pos, ch)
                while wch_issued[0] < min(len(wch_seq), pos + 1 + WCH_AHEAD):
                    j = wch_issued[0]
                    dma("pool", wch[j % 4].v(), D_(d_win[wch_seq[j]]))
                    wch_issued[0] += 1
                wch_pos[0] += 1
                return wch[pos % 4]

            def inproj_fm(ch):
                w = load_wchunk(ch)
                bks = [next_bank() for _ in range(NS)]
                for k in range(8):
                    for n in range(NS):
                        mm(PSF[bks[n]].v(), w.v(k), xT.v(k, slice(n * 512, (n + 1) * 512)), k == 0, k == 7)
                return bks

            bks_next = inproj_fm(0)
            for g in range(4):
                wdw = 2 ** (g + 1)
                bks = bks_next
                for n in range(NS):
                    act(uT.v(slice(8 + n * 512, 8 + (n + 1) * 512)), PSF[bks[n]].v(), AF.Identity,
                        bias=b_in.v(slice(g, g + 1)))
                W_ = PADW
                src = uT
                lvl = [(1, sA), (2, sB), (4, sA), (8, sB)]
                tt("dve", sA.v(slice(1, W_)), uT.v(slice(0, W_ - 1)), uT.v(slice(1, W_)), ALU.add)
                cur = sA
                lo, hi = 1, W_
                half = 1
                for li in range(1, g + 1):
                    dst = sB if cur is sA else sA
                    nlo, nhi = lo + half, hi - half
                    tt("dve", dst.v(slice(nlo, nhi)), cur.v(slice(nlo - half, nhi - half)),
                       cur.v(slice(nlo + half, nhi + half)), ALU.add)
                    cur = dst
                    lo, hi = nlo, nhi
                    half *= 2
                assert lo <= 8 and hi >= S_LEN + 8
                ts("dve", cur.v(slice(8, 8 + S_LEN)), cur.v(slice(8, 8 + S_LEN)), 1.0 / wdw, ALU.mult)
                tt("dve", cur.v(slice(8, 16)), cur.v(slice(8, 16)), ec.v(g, slice(0, 8)), ALU.mult)
                tt("dve", cur.v(slice(S_LEN, S_LEN + 8)), cur.v(slice(S_LEN, S_LEN + 8)), ec.v(g, slice(8, 16)), ALU.mult)
                tt("dve", pooled.v(), cur.v(slice(8, 8 + S_LEN)), uT.v(slice(8, 8 + S_LEN)), ALU.subtract)
                if g + 1 < 4:
                    bks_next = inproj_fm(g + 1)
                for n in range(NS):
                    b = next_bank()
                    mm(PSF[b].v(), wmix.v(g), pooled.v(slice(n * 512, (n + 1) * 512)), True, True)
                    act(mixT.v(g, slice(n * 512, (n + 1) * 512)), PSF[b].v(), AF.Identity,
                        bias=bms.v(slice(g, g + 1)), scale=pscale.v(slice(g, g + 1)))

            if stop <= 1:
                return
            def rope_chunk(ch_main, ch_swap, dst_fn):
                bk_m = inproj_fm(ch_main)
                bk_s = inproj_fm(ch_swap)
                for n in range(NS):
                    cs = slice(n * 512, (n + 1) * 512)
                    stt(rtmp1.v(), PSF[bk_m[n]].v(), b_in.v(slice(ch_main, ch_main + 1)), cosT.v(cs), ALU.add, ALU.mult)
                    stt(rtmp2.v(), PSF[bk_s[n]].v(), b_in.v(slice(ch_swap, ch_swap + 1)), sinT.v(cs), ALU.add, ALU.mult)
                    tt("pool", dst_fn(cs), rtmp1.v(), rtmp2.v(), ALU.add)

            for c in range(4):
                rope_chunk(4 + c, 8 + c, lambda cs, c=c: qT.v(c, cs))
            rope_chunk(12, 13, lambda cs: kT.v(cs))
            wv = load_wchunk(14)
            for t in range(NT):
                b = next_bank()
                pv = PSF[b]
                for k in range(8):
                    mm(pv.v(slice(0, 128)), xT.v(k, slice(t * 128, (t + 1) * 128)), wv.v(k), k == 0, k == 7)
                for h in range(2):
                    tt("dve", vaug.v(t, h, slice(0, 64)), pv.v(slice(h * 64, (h + 1) * 64)),
                       bv_bc.v(slice(h * 64, (h + 1) * 64)), ALU.add)

            if stop <= 2:
                return
            for i in range(NT):
                ot = otok[i % 2]
                qs = slice(i * 128, (i + 1) * 128)
                for g in range(2):
                    pg = slice(64 * g, 64 * g + 64)
                    pt = pT[(2 * i + g) % 2]
                    js = [j for j in (i - 1, i, i + 1) if 0 <= j < NT]
                    for jj, j in enumerate(js):
                        b = next_bank()
                        for hh in range(4):
                            mm(PSF[b].v(slice(hh * 128, (hh + 1) * 128)),
                               kT.v(slice(j * 128, (j + 1) * 128), p=pg), qT.v(hh, qs, p=pg), True, True)
                        act(pt.v(jj), PSF[b].v(), AF.Exp, scale=0.125)
                        if j == i - 1:
                            S.op("pool", lambda e, v=pt.v(jj): e.affine_select(
                                out=v.ap, in_=v.ap, pattern=[[0, 4], [-1, 128]], compare_op=ALU.is_ge,
                                fill=0.0, base=0, channel_multiplier=1), reads=[pt.v(jj)], writes=[pt.v(jj)])
                        elif j == i + 1:
                            S.op("pool", lambda e, v=pt.v(jj): e.affine_select(
                                out=v.ap, in_=v.ap, pattern=[[0, 4], [1, 128]], compare_op=ALU.is_ge,
                                fill=0.0, base=0, channel_multiplier=-1), reads=[pt.v(jj)], writes=[pt.v(jj)])
                    bo = next_bank()
                    po = psbuf(bo, [4, 65])
                    for hh in range(4):
                        for jj, j in enumerate(js):
                            mm(po.v(hh), pt.v(jj, slice(hh * 128, (hh + 1) * 128)), vaug.v(j, g),
                               jj == 0, jj == len(js) - 1)
                    tt("dve", den.v(), po.v(slice(0, 4), 64), esink.v(slice(4 * g, 4 * g + 4)), ALU.add)
                    S.op("dve", lambda e: e.reciprocal(out=rden.v().ap, in_=den.v().ap), reads=[den.v()], writes=[rden.v()])
                    for hh in range(4):
                        hd = 4 * g + hh
                        ts("dve", ot.v(slice(hd * 64, (hd + 1) * 64)), po.v(hh, slice(0, 64)),
                           rden.v(slice(hh, hh + 1)), ALU.mult)
                bt = next_bank()
                ptb = psbuf(bt, [4, 256], BF16)
                for c in range(4):
                    tr(ptb.v(c, slice(0, 128)), ot.v(slice(c * 128, (c + 1) * 128)), ident_b.v())
                cp("act", oT.v(slice(0, 4), qs), ptb.v(slice(0, 4), slice(0, 128)))

            if stop <= 3:
                return
            dma("pool", wpb.v(), D_(d_wpb))
            dma("pool", wab.v(), D_(d_wab))
            dma("pool", wo.v(), D_(d_wo))
            dma("sp", ln1g.v(), D_(d_ln1g))
            dma("sp", ln1b.v(), D_(d_ln1b))

            def layer_norm(a, stats_b, mv_b, rstd_b, g_b, b_b, out, mul_eng="pool", nbias_b=None):
                for c in range(2):
                    S.op("dve", lambda e, c=c: e.bn_stats(out=stats_b.v(c).ap, in_=a.v(slice(c * 512, (c + 1) * 512)).ap),
                         reads=[a.v(slice(c * 512, (c + 1) * 512))], writes=[stats_b.v(c)])
                S.op("dve", lambda e: e.bn_aggr(out=mv_b.v().ap, in_=stats_b.v().ap), reads=[stats_b.v()], writes=[mv_b.v()])
                act(rstd_b.v(), mv_b.v(slice(1, 2)), AF.Sqrt, bias=LN_EPS)
                S.op("dve", lambda e: e.reciprocal(out=rstd_b.v().ap, in_=rstd_b.v().ap), reads=[rstd_b.v()], writes=[rstd_b.v()])
                if nbias_b is None:
                    ts("dve", a.v(), a.v(), mv_b.v(slice(0, 1)), ALU.subtract, rstd_b.v(), ALU.mult)
                else:
                    ts("dve", nbias_b.v(), mv_b.v(slice(0, 1)), rstd_b.v(), ALU.mult, -1.0, ALU.mult)
                    act(a.v(), a.v(), AF.Identity, bias=nbias_b.v(), scale=rstd_b.v())
                tt(mul_eng, a.v(), a.v(), g_b.v(), ALU.mult)
                tt("dve", out.v(), a.v(), b_b.v(), ALU.add)

            def stage_A(n):
                cs = slice(n * 512, (n + 1) * 512)
                for m in range(8):
                    ms = slice(m * 128, (m + 1) * 128)
                    w0 = load_wchunk(15 + m)
                    w1 = load_wchunk(23 + m)
                    byp, bya, bg0, bg1 = next_bank(), next_bank(), next_bank(), next_bank()
                    for k in range(4):
                        mm(PSF[byp].v(), wpb.v(k, ms), mixT.v(k, cs), k == 0, k == 3)
                    for k in range(4):
                        mm(PSF[bya].v(), wab.v(k, ms), oT.v(k, cs), k == 0, k == 3)
                    for k in range(8):
                        mm(PSF[bg0].v(), w0.v(k), xT.v(k, cs), k == 0, k == 7)
                    for k in range(8):
                        mm(PSF[bg1].v(), w1.v(k), xT.v(k, cs), k == 0, k == 7)
                    act(sg0.v(), PSF[bg0].v(), AF.Sigmoid, bias=b_in.v(slice(15 + m, 16 + m)))
                    act(sg1.v(), PSF[bg1].v(), AF.Sigmoid, bias=b_in.v(slice(23 + m, 24 + m)))
                    tt("dve", t0b.v(), PSF[byp].v(), sg0.v(), ALU.mult)
                    tt("dve", t1b.v(), PSF[bya].v(), sg1.v(), ALU.mult)
                    tt("pool", mT.v(m), t0b.v(), t1b.v(), ALU.add)

            def stage_B(n):
                for tl in range(4):
                    t = n * 4 + tl
                    dma("sp", xtok[t % 4].v(), D_(d_x[t * 128:(t + 1) * 128, :]))
                for tl in range(4):
                    t = n * 4 + tl
                    xt = xtok[t % 4]
                    ab = abuf[t % 2]
                    for hf in range(2):
                        b = next_bank()
                        for m in range(8):
                            mm(PSF[b].v(), mT.v(m, slice(tl * 128, (tl + 1) * 128)), wo.v(m, slice(hf * 512, (hf + 1) * 512)),
                               m == 0, m == 7)
                        stt(ab.v(slice(hf * 512, (hf + 1) * 512)), xt.v(slice(hf * 512, (hf + 1) * 512)), ALPHA,
                            PSF[b].v(), ALU.mult, ALU.add)
                    layer_norm(ab, stats, mv, rstd, ln1g, ln1b, xt)
                    dma("sp", View(d_x2s[t * 128:(t + 1) * 128, :], "x2s", [(t, t + 1)]), xt.v())

            def stage_C(n):
                for tl in range(4):
                    t = n * 4 + tl
                    xt = xtok[t % 4]
                    b0, b1 = next_bank(), next_bank()
                    for k in range(8):
                        bb = b0 if k < 4 else b1
                        kk = k % 4
                        tr(PSF[bb].v(slice(kk * 128, (kk + 1) * 128)), xt.v(slice(k * 128, (k + 1) * 128)), ident_f.v())
                    for hb, bb in enumerate((b0, b1)):
                        pview = psbuf(bb, [4, 128])
                        cp("act", x2Tf.v(slice(hb * 4, hb * 4 + 4)), pview.v())
                    br_ = next_bank()
                    for k in range(8):
                        mm(PSF[br_].v(slice(0, 36)), x2Tf.v(k), wr_f.v(k), k == 0, k == 7)
                    tt("dve", lg.v(), PSF[br_].v(slice(0, 36)), br_bc.v(), ALU.add)
                    R = lambda a, b=None: rt.v(slice(a, (a + 1) if b is None else b))
                    S.op("dve", lambda e: e.tensor_reduce(out=R(0).ap, in_=lg.v(slice(0, 4)).ap, axis=AX.X, op=ALU.max, negate=True),
                         reads=[lg.v(slice(0, 4))], writes=[R(0)])
                    act(R(7, 11), lg.v(slice(0, 4)), AF.Exp, bias=R(0), accum=R(1))
                    S.op("dve", lambda e: e.reciprocal(out=R(2).ap, in_=R(1).ap), reads=[R(1)], writes=[R(2)])
                    ts("dve", R(3, 7), lg.v(slice(0, 4)), R(0), ALU.add, 0.0, ALU.is_ge)
                    ts("dve", R(11, 19), lg.v(slice(4, 12)), R(3), ALU.mult)
                    for gg in range(1, 4):
                        stt(R(11, 19), lg.v(slice(4 + 8 * gg, 12 + 8 * gg)), R(3 + gg), R(11, 19), ALU.mult, ALU.add)
                    S.op("dve", lambda e: e.tensor_reduce(out=R(19).ap, in_=R(11, 19).ap, axis=AX.X, op=ALU.max),
                         reads=[R(11, 19)], writes=[R(19)])
                    ts("dve", R(20, 28), R(11, 19), R(19), ALU.is_ge)
                    stt(R(28, 36), R(20, 28), -1e30, R(11, 19), ALU.mult, ALU.add)
                    S.op("dve", lambda e: e.tensor_reduce(out=R(36).ap, in_=R(28, 36).ap, axis=AX.X, op=ALU.max),
                         reads=[R(28, 36)], writes=[R(36)])
                    ts("dve", R(37, 45), R(28, 36), R(36), ALU.is_ge)
                    tt("dve", R(45), R(36), R(19), ALU.subtract)
                    act(R(46), R(45), AF.Exp)
                    ts("dve", R(47), R(46), 1.0, ALU.add)
                    S.op("dve", lambda e: e.reciprocal(out=R(47).ap, in_=R(47).ap), reads=[R(47)], writes=[R(47)])
                    tt("dve", R(48), R(47), R(2), ALU.mult)
                    tt("dve", R(49), R(48), R(46), ALU.mult)
                    ts("dve", R(50, 58), R(20, 28), R(48), ALU.mult)
                    stt(R(50, 58), R(37, 45), R(49), R(50, 58), ALU.mult, ALU.add)
                    for gg in range(4):
                        ts("dve", coef.v(t, slice(8 * gg, 8 * gg + 8)), R(50, 58), R(3 + gg), ALU.mult)
                        ts("dve", OH1f.v(t, slice(8 * gg, 8 * gg + 8)), R(20, 28), R(3 + gg), ALU.mult)
                        ts("dve", OH2f.v(t, slice(8 * gg, 8 * gg + 8)), R(37, 45), R(3 + gg), ALU.mult)
                    cp("dve", w12.v(0, slice(t, t + 1)), R(48))
                    cp("dve", w12.v(1, slice(t, t + 1)), R(49))

            stage_A(0)
            for n in range(NS):
                stage_B(n)
                if n + 1 < NS:
                    stage_A(n + 1)
                stage_C(n)

            if stop <= 4.5:
                return
            def load_w_static(s_):
                wg_b, wu_b, wd_b = wexp[s_ % NWS]
                dma("pool", wg_b.v(), D_(d_wg[s_]))
                dma("pool", wu_b.v(), D_(d_wu[s_]))
                dma("pool", wd_b.v(), D_(d_wd[s_]))
            if stop >= 5:
                for s_ in range(NWS):
                    load_w_static(s_)
            tt("dve", ind.v(), OH1f.v(), OH2f.v(), ALU.add)
            memset("pool", ones_bf.v(), 1.0)
            memset("pool", stri_bf.v(), 1.0)
            S.op("pool", lambda e: e.affine_select(out=stri_bf.v().ap, in_=stri_bf.v().ap, pattern=[[1, 128]],
                                                   compare_op=ALU.is_ge, fill=0.0, base=-1, channel_multiplier=-1),
                 reads=[stri_bf.v()], writes=[stri_bf.v()])
            memset("dve", zeros32.v(), 0.0)
            S.op("pool", lambda e: e.iota(pidx_i.v().ap, [[0, 1]], base=0, channel_multiplier=1), writes=[pidx_i.v()])
            cp("dve", pidx.v(), pidx_i.v())
            pb, cb = next_bank(), next_bank()
            for i in range(NT):
                for j in range(i + 1):
                    mm(PSF[pb].v(slice(i * 32, (i + 1) * 32)), (ones_bf if j < i else stri_bf).v(), ind.v(j), j == 0, j == i)
            for j in range(NT):
                mm(PSF[cb].v(slice(0, 32)), ones_bf.v(), ind.v(j), j == 0, j == NT - 1)
            cp("dve", prefix.v(), psbuf(pb, [NT, 32]).v())
            cp("dve", cnt.v(), PSF[cb].v(slice(0, 32)))
            ts("dve", cm_f.v(), cnt.v(), -float(SLOT_B), ALU.add, 0.0, ALU.max)
            cp("dve", cnt_i.v(), cm_f.v())
            ts("dve", nb_i.v(), cnt_i.v(), SLOT_B - 1, ALU.add)
            ts("dve", nb_i.v(), nb_i.v(), SLOT_SHIFT, ALU.arith_shift_right)
            cp("dve", nb_f.v(), nb_i.v())
            S.op("dve", lambda e: e.tensor_tensor_scan(out=pendb.v().ap, data0=zeros32.v().ap, data1=nb_f.v().ap,
                                                       initial=0.0, op0=ALU.add, op1=ALU.add),
                 reads=[zeros32.v(), nb_f.v()], writes=[pendb.v()])
            S.op("pool", lambda e: e.iota(basep_i.v().ap, [[SLOT_B, N_EXP]], base=0, channel_multiplier=0),
                 writes=[basep_i.v()])
            cp("dve", basep.v(), basep_i.v())
            tt("dve", pstart.v(), pendb.v(), nb_f.v(), ALU.subtract)
            ts("dve", pstart.v(), pstart.v(), float(SLOT_B), ALU.mult, float(N_EXP * SLOT_B - SLOT_B), ALU.add)
            tt("dve", pstart.v(), pstart.v(), basep.v(), ALU.subtract)
            for s_ in range(N_OVF):
                S.op("dve", lambda e, s_=s_: e.tensor_scalar(out=junk32.v().ap, in0=pendb.v().ap, scalar1=float(s_), scalar2=0.0,
                                                             op0=ALU.is_le, op1=ALU.add, accum_out=es_f.v(slice(s_, s_ + 1)).ap),
                     reads=[pendb.v()], writes=[junk32.v(), es_f.v(slice(s_, s_ + 1))])
            ts("dve", widx_f.v(slice(0, N_OVF)), es_f.v(slice(0, N_OVF)), 128.0, ALU.mult, pidx.v(), ALU.add)
            cp("dve", widx_i.v(slice(0, N_OVF)), widx_f.v(slice(0, N_OVF)))
            for t in range(NT):
                ts("dve", slotpos.v(t), prefix.v(t), float(SLOT_B), ALU.is_ge)
                tt("dve", slotpos.v(t), slotpos.v(t), pstart.v(), ALU.mult)
                tt("dve", slotpos.v(t), slotpos.v(t), prefix.v(t), ALU.add)
                tt("dve", slotpos.v(t), slotpos.v(t), basep.v(), ALU.add)
            for (ohf, dfl, din_) in ((OH1f, d1f, d1i), (OH2f, d2f, d2i)):
                tt("dve", tmp512.v(), ohf.v(), slotpos.v(), ALU.mult)
                S.op("dve", lambda e, dfl=dfl: e.tensor_reduce(out=dfl.v().ap, in_=tmp512.v().ap, axis=AX.X, op=ALU.add),
                     reads=[tmp512.v()], writes=[dfl.v()])
                cp("dve", din_.v(), dfl.v())
            if stop <= 4.8:
                return
            for t in range(NT):
                xr_ = xr[t % 4]
                dma("sp", xr_.v(), View(d_x2s[t * 128:(t + 1) * 128, :], "x2s", [(t, t + 1)]))
                for j, din_ in enumerate((d1i, d2i)):
                    S.op("pool", lambda e, xr_=xr_, din_=din_, t=t: e.indirect_dma_start(
                        out=d_xin[:, :], out_offset=bass.IndirectOffsetOnAxis(ap=din_.v(slice(t, t + 1)).ap, axis=0),
                        in_=xr_.v().ap, in_offset=None),
                        reads=[xr_.v(), din_.v(slice(t, t + 1))], writes=[View(None, "xin", [(2 * t + j, 2 * t + j + 1)])], dma=True)
            if stop <= 4.9:
                return
            _bc = {}

            def _bc_reg(e):
                if "r" not in _bc:
                    _bc["r"] = e.to_reg(N_EXP * 128 - 1)
                return _bc["r"]
            xin_all = View(None, "xin", [(0, 2 * NT)])
            NQ = N_SLOTS * SUBS

            order = list(range(NWS))
            _ip, _io = NWS, N_EXP
            for ch_ in "PPOPPOPPOPO" * 4:
                if ch_ == "P":
                    order.append(_ip)
                    _ip += 1
                else:
                    order.append(_io)
                    _io += 1
            assert sorted(order) == list(range(N_SLOTS)), order

            def load_w(pos_):
                s_ = order[pos_]
                if s_ < N_EXP:
                    wg_b, wu_b, wd_b = wexp[pos_ % NWS]
                    dma("pool", wg_b.v(), D_(d_wg[s_]))
                    dma("pool", wu_b.v(), D_(d_wu[s_]))
                    dma("pool", wd_b.v(), D_(d_wd[s_]))
                    return
                wg2, wu2, wd2 = wexp2[pos_ % NWS]
                s_ = s_ - N_EXP
                for (w2_, drows) in ((wg2, d_wg_rows), (wu2, d_wu_rows), (wd2, d_wd_rows)):
                    S.op("pool", lambda e, w2_=w2_, drows=drows, s_=s_: e.indirect_dma_start(
                        out=w2_.v().ap, out_offset=None, in_=drows,
                        in_offset=bass.IndirectOffsetOnAxis(ap=widx_i.v(slice(s_, s_ + 1)).ap, axis=0),
                        bounds_check=_bc_reg(e), oob_is_err=False),
                        reads=[widx_i.v(slice(s_, s_ + 1))], writes=[w2_.v()], dma=True)

            def stage_L(qs):
                q = order[qs // SUBS] * SUBS + qs % SUBS
                r0 = q * 128
                xb_ = xb[qs % 4]
                S.op("act", lambda e, xb_=xb_, r0=r0: e.dma_start(out=xb_.v().ap, in_=d_xin[r0:r0 + 128, :]),
                     reads=[xin_all], writes=[xb_.v()], dma=True)

            def stage_T(qs):
                xb_ = xb[qs % 4]
                xbT_ = xbT[qs % 4]
                for hb in range(2):
                    bt = next_bank()
                    ptb = psbuf(bt, [4, 256], BF16)
                    for kk in range(4):
                        k = hb * 4 + kk
                        tr(ptb.v(kk, slice(0, 128)), xb_.v(slice(k * 128, (k + 1) * 128)), ident_b.v())
                    cp("act" if hb == 0 else "dve", xbT_.v(slice(hb * 4, hb * 4 + 4)), ptb.v(slice(0, 4), slice(0, 128)))

            def stage_G(qs):
                wg_b, wu_b, wd_b = wexp[(qs // SUBS) % NWS]
                xbT_ = xbT[qs % 4]
                hT_ = hTs[qs % 4]
                bg, bu = next_bank(), next_bank()
                for f in range(4):
                    fs = slice(f * 128, (f + 1) * 128)
                    for k in range(8):
                        mm(PSF[bg].v(fs), wg_b.v(k, fs), xbT_.v(k), k == 0, k == 7)
                for f in range(4):
                    fs = slice(f * 128, (f + 1) * 128)
                    for k in range(8):
                        mm(PSF[bu].v(fs), wu_b.v(k, fs), xbT_.v(k), k == 0, k == 7)
                s_l = sl[qs % 2]
                act(s_l.v(), PSF[bg].v(), AF.Silu)
                tt("dve", hT_.v(), PSF[bu].v(), s_l.v(), ALU.mult)

            def stage_D(qs):
                q = order[qs // SUBS] * SUBS + qs % SUBS
                wg_b, wu_b, wd_b = wexp[(qs // SUBS) % NWS]
                hT4 = hTs4[qs % 4]
                yb_ = ybs[qs % 4]
                r0 = q * 128
                for hf in range(2):
                    b = next_bank()
                    for f in range(4):
                        mm(PSF[b].v(), hT4.v(f), wd_b.v(f, slice(hf * 512, (hf + 1) * 512)), f == 0, f == 3)
                    cp("act" if hf == 0 else "dve", yb_.v(slice(hf * 512, (hf + 1) * 512)), PSF[b].v())
                S.op("sp", lambda e, yb_=yb_, r0=r0: e.dma_start(out=d_yb[r0:r0 + 128, :], in_=yb_.v().ap),
                     reads=[yb_.v()], writes=[View(None, "yb", [(q, q + 1)])], dma=True)

            next_w = NWS
            XB_AHEAD = 2
            for it in range(NQ + 2):
                while next_w < N_SLOTS and (next_w < NWS or it >= (next_w - NWS) * SUBS + SUBS + 2):
                    load_w(next_w)
                    next_w += 1
                if it == 0:
                    for q_ in range(min(XB_AHEAD, NQ)):
                        stage_L(q_)
                if it + XB_AHEAD < NQ:
                    stage_L(it + XB_AHEAD)
                if it < NQ:
                    stage_T(it)
                if it >= 2:
                    stage_D(it - 2)
                if 1 <= it <= NQ:
                    stage_G(it - 1)
            assert next_w == N_SLOTS

            if stop <= 5:
                return
            yb_all = View(None, "yb", [(0, N_SLOTS * SUBS)])
            dma("sp", ln2g.v(), D_(d_ln2g))
            dma("sp", ln2b.v(), D_(d_ln2b))
            X2_AHEAD = 3

            def load_x2r(t):
                S.op("act", lambda e, t=t: e.dma_start(out=x2rb[t % 4].v().ap, in_=d_x2s[t * 128:(t + 1) * 128, :]),
                     reads=[View(None, "x2s", [(t, t + 1)])], writes=[x2rb[t % 4].v()], dma=True)
            for t in range(min(X2_AHEAD, NT)):
                load_x2r(t)
            for t in range(NT):
                xr_ = x2rb[t % 4]
                ab = a2[t % 2]
                g1, g2 = rg1[t % 2], rg2[t % 2]
                if t + X2_AHEAD < NT:
                    load_x2r(t + X2_AHEAD)
                for (gb, din_) in ((g1, d1i), (g2, d2i)):
                    S.op("pool", lambda e, gb=gb, din_=din_, t=t: e.indirect_dma_start(
                        out=gb.v().ap, out_offset=None, in_=d_yb[:, :],
                        in_offset=bass.IndirectOffsetOnAxis(ap=din_.v(slice(t, t + 1)).ap, axis=0)),
                        reads=[yb_all, din_.v(slice(t, t + 1))], writes=[gb.v()], dma=True)
                act(g1.v(), g1.v(), AF.Identity, scale=w12.v(0, slice(t, t + 1)))
                stt(g1.v(), g2.v(), w12.v(1, slice(t, t + 1)), g1.v(), ALU.mult, ALU.add)
                stt(ab.v(), xr_.v(), ALPHA, g1.v(), ALU.mult, ALU.add)
                layer_norm(ab, stats2, mv2, rstd2, ln2g, ln2b, ab, mul_eng="dve", nbias_b=nbias2)
                dma("sp", View(d_out[t * 128:(t + 1) * 128, :], "out", [(t, t + 1)]), ab.v())

        body()
        for (dv, bv_) in dbg_list:
            dma("sp", View(dv, "dbgout", [(0, 1)]), bv_)
        S.emit(es)
    return nc


def _prep_shared(inp, with_experts=True):
    f = np.float32
    w_in = np.asarray(inp["w_in"], f)[0]
    b_in = np.asarray(inp["b_in"], f)[0]
    cols = []
    for g in range(4):
        cols.append(np.arange(g * 128, (g + 1) * 128))
    d = np.arange(64)
    sw = (d + 32) % 64
    for c in range(4):
        cols.append(np.concatenate([512 + c * 64 + d, 512 + (4 + c) * 64 + d]))
    for c in range(4):
        cols.append(np.concatenate([512 + c * 64 + sw, 512 + (4 + c) * 64 + sw]))
    cols.append(np.concatenate([1024 + d, 1024 + 64 + d]))
    cols.append(np.concatenate([1024 + sw, 1024 + 64 + sw]))
    cols.append(np.arange(1152, 1280))
    for m in range(16):
        cols.append(np.arange(1280 + m * 128, 1280 + (m + 1) * 128))
    cols = np.stack(cols)
    assert cols.shape == (N_INCH, 128)
    wsel = w_in[:, cols]
    w_in_r = np.ascontiguousarray(wsel.reshape(8, 128, N_INCH, 128).transpose(2, 1, 0, 3))
    b_in_r = np.ascontiguousarray(b_in[cols].T)
    bv_bc = np.ascontiguousarray(np.broadcast_to(b_in[1152:1280][None, :], (128, 128)))
    pos = np.arange(S_LEN, dtype=f)
    inv = (f(10000.0) ** (-np.arange(0, 64, 2, dtype=f) / f(64))).astype(f)
    ang = (pos[:, None] * inv[None, :]).astype(f)
    cos = np.cos(ang).astype(f)
    sin = np.sin(ang).astype(f)
    p = np.arange(128)
    fi = (p % 64) % 32
    sign = np.where((p % 64) < 32, -1.0, 1.0).astype(f)
    cosT = np.ascontiguousarray(cos[:, fi].T)
    sinT = np.ascontiguousarray((sin[:, fi] * sign[None, :]).T.astype(f))
    ec = np.ones((128, 4, 16), f)
    tpos = np.concatenate([np.arange(8), np.arange(S_LEN - 8, S_LEN)])
    for g, w in enumerate((2, 4, 8, 16)):
        lo = np.clip(tpos - w // 2, 0, S_LEN)
        hi = np.clip(tpos - w // 2 + w, 0, S_LEN)
        ec[:, g, :] = (f(w) / (hi - lo).astype(f))[None, :]
    rep = lambda v, n=128: np.ascontiguousarray(np.broadcast_to(np.asarray(v, f)[None, :], (n, len(v))))
    sh = {
        "w_in_r": w_in_r, "b_in_r": b_in_r, "bv_bc": bv_bc, "cosT": cosT, "sinT": sinT, "ec": ec,
        "wmix_r": np.ascontiguousarray(np.asarray(inp["w_pool_mix"], f)[0].transpose(1, 0, 2)),
        "bmix_r": np.ascontiguousarray(np.asarray(inp["b_pool_mix"], f)[0].T),
        "pscale_r": np.ascontiguousarray(np.asarray(inp["pool_scale"], f)[0].reshape(4, 128).T),
        "sink_bc": rep(np.asarray(inp["attn_sink"], f)[0]),
        "wpb_r": np.ascontiguousarray(np.asarray(inp["w_pool_br"], f)[0].reshape(4, 128, D).transpose(1, 0, 2)),
        "wab_r": np.ascontiguousarray(np.asarray(inp["w_attn_br"], f)[0].reshape(4, 128, D).transpose(1, 0, 2)),
        "wo_r": np.ascontiguousarray(np.asarray(inp["w_o"], f)[0].reshape(8, 128, D).transpose(1, 0, 2)),
        "ln1g_bc": rep(np.asarray(inp["ln1_g"], f)[0]), "ln1b_bc": rep(np.asarray(inp["ln1_b"], f)[0]),
        "ln2g_bc": rep(np.asarray(inp["ln2_g"], f)[0]), "ln2b_bc": rep(np.asarray(inp["ln2_b"], f)[0]),
        "wr_r": np.ascontiguousarray(np.concatenate([np.asarray(inp["w_router_group"], f)[0],
                                                     np.asarray(inp["w_router_expert"], f)[0]], axis=1)
                                     .reshape(8, 128, 36).transpose(1, 0, 2)),
        "br_bc": rep(np.concatenate([np.asarray(inp["b_router_group"], f)[0], np.asarray(inp["b_router_expert"], f)[0]])),
    }
    if with_experts:
        sh.update({
        "wg_r": np.ascontiguousarray(np.asarray(inp["w_gate"], f)[0].reshape(N_EXP, 8, 128, 512).transpose(0, 2, 1, 3)),
        "wu_r": np.ascontiguousarray(np.asarray(inp["w_up"], f)[0].reshape(N_EXP, 8, 128, 512).transpose(0, 2, 1, 3)),
        "wd_r": np.ascontiguousarray(np.asarray(inp["w_down"], f)[0].reshape(N_EXP, 4, 128, D).transpose(0, 2, 1, 3)),
        })
    return sh


_NC_CACHE = {}


def run_debug(inputs, stop, n_cores=N_CORES):
    x = np.asarray(inputs["x"], np.float32)
    sh = _prep_shared(inputs, with_experts=(stop >= 5))
    nc = build_program(stop=stop, dbg=True)
    in_maps = []
    for c in range(n_cores):
        m = dict(sh)
        m["x_tok"] = np.ascontiguousarray(x[c])
        m["xT"] = np.ascontiguousarray(x[c].T)
        in_maps.append(m)
    res = run_bass_kernel_spmd(nc, in_maps, core_ids=list(range(n_cores)))
    return res.results


def kernel(**inputs):
    x = np.asarray(inputs["x"], np.float32)
    sh = _prep_shared(inputs)
    if "nc" not in _NC_CACHE:
        _NC_CACHE["nc"] = build_program()
    nc = _NC_CACHE["nc"]
    in_maps = []
    for c in range(N_CORES):
        m = dict(sh)
        m["x_tok"] = np.ascontiguousarray(x[c])
        m["xT"] = np.ascontiguousarray(x[c].T)
        in_maps.append(m)
    res = run_bass_kernel_spmd(nc, in_maps, core_ids=list(range(N_CORES)))
    out = np.stack([np.asarray(res.results[c]["out"], np.float32) for c in range(N_CORES)], axis=0)
    return out
```
